# Optimizing a Trainium2 kernel written in Bass

```python
import numpy as np
import jax
import jax.numpy as jnp
from jax import lax

D_MODEL = 2048
BATCH = 1
SEQ = 8192
DEPTH = 4

CHUNK = 64
EPS = 1e-6

HG_HEADS = 8
HG_DK = 128
HG_DV = 128
HG_WIDTH = HG_HEADS * HG_DV
HG_COLS = (HG_HEADS * HG_DK, HG_HEADS * HG_DK, HG_WIDTH, HG_WIDTH)

RW_HEADS = 16
RW_N = 64
RW_WIDTH = RW_HEADS * RW_N
RW_DECAY_LORA = 64
RW_AAA_LORA = 64
RW_GATE_LORA = 160
RW_GN_EPS = 64e-5
RW_COLS = (RW_WIDTH, RW_WIDTH, RW_WIDTH, RW_DECAY_LORA, RW_AAA_LORA, RW_GATE_LORA)

ML_HEADS = 4
ML_DQK = 128
ML_DV = 256
ML_GATE_CAP = 15.0
ML_COLS = (ML_HEADS * ML_DQK, ML_HEADS * ML_DQK, ML_HEADS * ML_DV, ML_HEADS, ML_HEADS, ML_HEADS * ML_DV)

MB_HEADS = 16
MB_HEADDIM = 64
MB_D_INNER = MB_HEADS * MB_HEADDIM
MB_D_STATE = 128
MB_GROUPS = 2
MB_CONV = 4
MB_XBC = MB_D_INNER + 2 * MB_GROUPS * MB_D_STATE
MB_COLS = (MB_D_INNER, MB_XBC, MB_HEADS)

E_IN_EVEN = sum(HG_COLS) + sum(RW_COLS)
E_IN_ODD = sum(ML_COLS) + sum(MB_COLS)
MIX_WIDTH_EVEN = HG_WIDTH + RW_WIDTH
MIX_WIDTH_ODD = ML_HEADS * ML_DV + MB_D_INNER
D_FF = -((-8 * D_MODEL) // (3 * 256)) * 256

kernel_name = 'hybrid_hgrn2_rwkv7_mlstm_mamba2_trunk'


def rms_norm(x, g):
    xf = x.astype(jnp.float32)
    y = xf * lax.rsqrt(jnp.mean(xf * xf, axis=-1, keepdims=True) + EPS)
    return (y * g).astype(x.dtype)


def head_rmsnorm(t, n_heads, g):
    b, s, w = t.shape
    th = t.reshape(b, s, n_heads, w // n_heads)
    th = th * lax.rsqrt(jnp.mean(th * th, axis=-1, keepdims=True) + EPS)
    return th.reshape(b, s, w) * g


def head_layernorm(t, w, bias, eps):
    mu = jnp.mean(t, axis=-1, keepdims=True)
    var = jnp.mean(jnp.square(t - mu), axis=-1, keepdims=True)
    y = (t - mu) * lax.rsqrt(var + eps)
    return y.reshape(t.shape[0], t.shape[1], -1) * w + bias


def split_cols(p, sizes):
    return jnp.split(p, np.cumsum(sizes)[:-1].tolist(), axis=-1)


def token_shift(p):
    return jnp.pad(p, ((0, 0), (1, 0), (0, 0)))[:, :-1]


def causal_dwconv(x, w, bias):
    y = lax.conv_general_dilated(x, w.astype(x.dtype)[:, None, :], window_strides=(1,),
                                 padding=[(w.shape[0] - 1, 0)],
                                 dimension_numbers=('NWC', 'WIO', 'NWC'),
                                 feature_group_count=x.shape[-1])
    return y + bias


def causal_mask():
    return jnp.tril(jnp.ones((CHUNK, CHUNK), dtype=bool))


def chunk_seq(t, n_feat=1):
    b, s = t.shape[:2]
    t = t.reshape(b, s // CHUNK, CHUNK, *t.shape[2:])
    t = jnp.moveaxis(t, 2, t.ndim - 1 - n_feat)
    return jnp.moveaxis(t, 1, 0)


def unchunk_seq(t):
    t = jnp.moveaxis(t, 0, 1)
    t = jnp.moveaxis(t, -2, 2)
    b, nc, l = t.shape[:3]
    return t.reshape(b, nc * l, -1)


def hgrn2_chunked(qc, kc, vc, gc):
    mask = causal_mask()

    def step(state, inp):
        q_, k_, v_, g_ = inp
        bcum = jnp.cumsum(g_, axis=-2)
        rel = jnp.where(mask[:, :, None], bcum[..., :, None, :] - bcum[..., None, :, :], -jnp.inf)
        scores = jnp.einsum('bhtd,bhsd,bhtsd->bhts', q_, k_, jnp.exp(rel))
        o = (jnp.einsum('bhts,bhsv->bhtv', scores, v_)
             + jnp.einsum('bhtd,bhdv->bhtv', q_ * jnp.exp(bcum), state))
        b_last = bcum[..., -1:, :]
        state = (jnp.exp(b_last[..., 0, :])[..., None] * state
                 + jnp.einsum('bhsd,bhsv->bhdv', k_ * jnp.exp(b_last - bcum), v_))
        return state, o

    _, b, n_h, _, dk = qc.shape
    state0 = jnp.zeros((b, n_h, dk, vc.shape[-1]), qc.dtype)
    _, out = lax.scan(step, state0, (qc, kc, vc, gc))
    return out


def rwkv7_scan(r, w, k, v, kk, a):
    b, _, n_h, n = r.shape

    def step(state, inp):
        rt, wt, kt, vt, kkt, at = inp
        sa = jnp.einsum('bhvk,bhk->bhv', state, -kkt)
        state = (state * wt[:, :, None, :] + sa[..., :, None] * (kkt * at)[:, :, None, :]
                 + vt[..., :, None] * kt[:, :, None, :])
        return state, jnp.einsum('bhvk,bhk->bhv', state, rt)

    xs = tuple(jnp.moveaxis(t, 1, 0) for t in (r, w, k, v, kk, a))
    state0 = jnp.zeros((b, n_h, n, n), r.dtype)
    _, out = lax.scan(step, state0, xs)
    return jnp.moveaxis(out, 0, 1)


def mlstm_chunked(qc, kc, vc, ic, fc):
    mask = causal_mask()

    def step(carry, inp):
        c_mat, n_vec, m = carry
        q_, k_, v_, i_, f_ = inp
        bcum = jnp.cumsum(f_, axis=-1)
        dmat = jnp.where(mask, bcum[..., :, None] - bcum[..., None, :] + i_[..., None, :], -jnp.inf)
        inter = bcum + m[..., None]
        m_t = jnp.maximum(inter, jnp.max(dmat, axis=-1))
        w_inter = jnp.exp(inter - m_t)
        pmat = jnp.exp(dmat - m_t[..., None]) * jnp.einsum('bhtd,bhsd->bhts', q_, k_)
        num = (jnp.einsum('bhts,bhsv->bhtv', pmat, v_)
               + w_inter[..., None] * jnp.einsum('bhtd,bhdv->bhtv', q_, c_mat))
        den = jnp.sum(pmat, axis=-1) + w_inter * jnp.einsum('bhtd,bhd->bht', q_, n_vec)
        h_out = num / jnp.maximum(jnp.abs(den), jnp.exp(-m_t))[..., None]
        b_last = bcum[..., -1]
        src = b_last[..., None] - bcum + i_
        m_new = jnp.maximum(b_last + m, jnp.max(src, axis=-1))
        ws = jnp.exp(src - m_new[..., None])
        carry_decay = jnp.exp(b_last + m - m_new)
        c_mat = carry_decay[..., None, None] * c_mat + jnp.einsum('bhs,bhsd,bhsv->bhdv', ws, k_, v_)
        n_vec = carry_decay[..., None] * n_vec + jnp.einsum('bhs,bhsd->bhd', ws, k_)
        return (c_mat, n_vec, m_new), h_out

    _, b, n_h, _, dqk = qc.shape
    dv = vc.shape[-1]
    init = (jnp.zeros((b, n_h, dqk, dv), qc.dtype), jnp.zeros((b, n_h, dqk), qc.dtype),
            jnp.zeros((b, n_h), qc.dtype))
    _, out = lax.scan(step, init, (qc, kc, vc, ic, fc))
    return out


def ssd_chunked(xc, dtc, dac, bc, cc):
    mask = causal_mask()

    def step(state, inp):
        x_, dt_, da_, b_, c_ = inp
        a = jnp.cumsum(da_, axis=-1)
        seg = jnp.exp(jnp.where(mask, a[..., :, None] - a[..., None, :], -jnp.inf))
        cb = jnp.einsum('bgtn,bgsn->bgts', c_, b_)
        scores = seg * cb[:, :, None] * dt_[..., None, :]
        y = (jnp.einsum('bgrts,bgrsp->bgrtp', scores, x_)
             + jnp.exp(a)[..., None] * jnp.einsum('bgtn,bgrpn->bgrtp', c_, state))
        a_last = a[..., -1:]
        ws = jnp.exp(a_last - a) * dt_
        state = (jnp.exp(a_last)[..., None] * state
                 + jnp.einsum('bgrs,bgrsp,bgsn->bgrpn', ws, x_, b_))
        return state, y

    _, b, g, r, _, p = xc.shape
    state0 = jnp.zeros((b, g, r, p, bc.shape[-1]), xc.dtype)
    _, out = lax.scan(step, state0, (xc, dtc, dac, bc, cc))
    return out


def hgrn2_rwkv7_mixer(u, layer, w_in, w_out, hg_lb_table, hg_norm, rw_mu, rw_w0, rw_w2, rw_a0, rw_a2,
                      rw_g2, rw_k_k, rw_k_a, rw_r_k, rw_ln_w, rw_ln_b):
    f32 = jnp.float32
    b, s, _ = u.shape
    p = (u @ w_in).astype(f32)
    n_hg = sum(HG_COLS)
    p_hg, p_rw = p[..., :n_hg], p[..., n_hg:]

    q, f, i, g = split_cols(p_hg, HG_COLS)
    lb = jnp.cumsum(jax.nn.softmax(hg_lb_table.astype(f32), axis=0), axis=0)
    lb = (lb - lb[0])[layer]
    log_f = jnp.logaddexp(jnp.log(lb), jnp.log1p(-lb) + jax.nn.log_sigmoid(f))
    k = (1.0 - lb) * jax.nn.sigmoid(-f)
    q = jax.nn.silu(q)
    o_hg = hgrn2_chunked(chunk_seq(q.reshape(b, s, HG_HEADS, HG_DK)), chunk_seq(k.reshape(b, s, HG_HEADS, HG_DK)),
                         chunk_seq(i.reshape(b, s, HG_HEADS, HG_DV)), chunk_seq(log_f.reshape(b, s, HG_HEADS, HG_DK)))
    o_hg = head_rmsnorm(unchunk_seq(o_hg), HG_HEADS, hg_norm) * jax.nn.sigmoid(g)

    p_rw = p_rw + (token_shift(p_rw) - p_rw) * rw_mu
    r, k, v, wl, al, gl = split_cols(p_rw, RW_COLS)
    w_log = -jax.nn.softplus(-(rw_w0 + jnp.tanh(wl) @ rw_w2)) - 0.5
    decay = jnp.exp(-jnp.exp(w_log))
    a = jax.nn.sigmoid(rw_a0 + al @ rw_a2)
    gate = jax.nn.sigmoid(gl) @ rw_g2
    heads = lambda t: t.reshape(b, s, RW_HEADS, RW_N)
    kk = heads(k * rw_k_k)
    kk = kk / jnp.maximum(jnp.sqrt(jnp.sum(kk * kk, axis=-1, keepdims=True)), 1e-12)
    k = k * (1.0 + (a - 1.0) * rw_k_a)
    rh, kh, vh = heads(r), heads(k), heads(v)
    o_rw = rwkv7_scan(rh, heads(decay), kh, vh, kk, heads(a))
    o_rw = head_layernorm(o_rw, rw_ln_w, rw_ln_b, RW_GN_EPS)
    bonus = (jnp.sum(rh * kh * rw_r_k, axis=-1, keepdims=True) * vh).reshape(b, s, RW_WIDTH)
    o_rw = (o_rw + bonus) * gate

    return jnp.concatenate([o_hg, o_rw], axis=-1).astype(u.dtype) @ w_out


def mlstm_mamba2_mixer(u, w_in, w_out, ml_i_bias, ml_f_bias, ml_norm, mb_conv_w, mb_conv_b, mb_dt_bias,
                       mb_A_log, mb_D, mb_norm):
    f32 = jnp.float32
    b, s, _ = u.shape
    p = (u @ w_in).astype(f32)
    n_ml = sum(ML_COLS)

    q, k, v, ig, fg, og = split_cols(p[..., :n_ml], ML_COLS)
    softcap = lambda t: ML_GATE_CAP * jnp.tanh(t / ML_GATE_CAP)
    i_pre = softcap(ig + ml_i_bias)
    log_f = jax.nn.log_sigmoid(softcap(fg + ml_f_bias))
    q = q * (ML_DQK ** -0.5)
    o_ml = mlstm_chunked(chunk_seq(q.reshape(b, s, ML_HEADS, ML_DQK)), chunk_seq(k.reshape(b, s, ML_HEADS, ML_DQK)),
                         chunk_seq(v.reshape(b, s, ML_HEADS, ML_DV)), chunk_seq(i_pre, 0), chunk_seq(log_f, 0))
    o_ml = head_rmsnorm(unchunk_seq(o_ml), ML_HEADS, ml_norm) * jax.nn.sigmoid(og)

    z, xbc, dt = split_cols(p[..., n_ml:], MB_COLS)
    xbc = jax.nn.silu(causal_dwconv(xbc, mb_conv_w, mb_conv_b))
    xm, bm, cm = split_cols(xbc, (MB_D_INNER, MB_GROUPS * MB_D_STATE, MB_GROUPS * MB_D_STATE))
    dt = jax.nn.softplus(dt + mb_dt_bias)
    d_a = -jnp.exp(mb_A_log.astype(f32)) * dt
    rep = MB_HEADS // MB_GROUPS
    y = ssd_chunked(chunk_seq(xm.reshape(b, s, MB_GROUPS, rep, MB_HEADDIM)),
                    chunk_seq(dt.reshape(b, s, MB_GROUPS, rep), 0), chunk_seq(d_a.reshape(b, s, MB_GROUPS, rep), 0),
                    chunk_seq(bm.reshape(b, s, MB_GROUPS, MB_D_STATE)), chunk_seq(cm.reshape(b, s, MB_GROUPS, MB_D_STATE)))
    y = unchunk_seq(y) + xm * jnp.repeat(mb_D, MB_HEADDIM)
    y = head_rmsnorm(y * jax.nn.silu(z), MB_GROUPS, mb_norm)

    return jnp.concatenate([o_ml, y], axis=-1).astype(u.dtype) @ w_out


def swiglu(h, w_up, w_down):
    gate, up = jnp.split(h @ w_up, 2, axis=-1)
    return (jax.nn.silu(gate) * up) @ w_down


def setup_inputs(seed: int = 0) -> dict:
    key = jax.random.key(seed)
    keys = iter(jax.random.split(key, 64))
    nrm = lambda shape, scale: scale * jax.random.normal(next(keys), shape, jnp.float32)
    uni = lambda shape, lo, hi: jax.random.uniform(next(keys), shape, jnp.float32, lo, hi)
    ne, no = (DEPTH + 1) // 2, DEPTH // 2
    dt0 = jnp.exp(uni((no, MB_HEADS), float(np.log(1e-3)), float(np.log(1e-1))))
    return {
        'x': nrm((BATCH, SEQ, D_MODEL), 1.0),
        'norm_mix': 1.0 + nrm((DEPTH, D_MODEL), 0.05),
        'norm_ffn': 1.0 + nrm((DEPTH, D_MODEL), 0.05),
        'norm_final': 1.0 + nrm((D_MODEL,), 0.05),
        'w_in_even': nrm((ne, D_MODEL, E_IN_EVEN), D_MODEL ** -0.5),
        'w_out_even': nrm((ne, MIX_WIDTH_EVEN, D_MODEL), 0.5 * MIX_WIDTH_EVEN ** -0.5),
        'hg_lb_table': nrm((DEPTH, HG_HEADS * HG_DK), 0.5),
        'hg_norm': 1.0 + nrm((ne, HG_WIDTH), 0.05),
        'rw_mu': uni((ne, sum(RW_COLS)), 0.0, 1.0),
        'rw_w0': -1.0 + nrm((ne, RW_WIDTH), 0.5),
        'rw_w2': nrm((ne, RW_DECAY_LORA, RW_WIDTH), 0.5 * RW_DECAY_LORA ** -0.5),
        'rw_a0': nrm((ne, RW_WIDTH), 0.1),
        'rw_a2': nrm((ne, RW_AAA_LORA, RW_WIDTH), RW_AAA_LORA ** -0.5),
        'rw_g2': nrm((ne, RW_GATE_LORA, RW_WIDTH), RW_GATE_LORA ** -0.5),
        'rw_k_k': 0.85 + nrm((ne, RW_WIDTH), 0.05),
        'rw_k_a': 1.0 + nrm((ne, RW_WIDTH), 0.05),
        'rw_r_k': nrm((ne, RW_HEADS, RW_N), 0.1),
        'rw_ln_w': 1.0 + nrm((ne, RW_WIDTH), 0.05),
        'rw_ln_b': nrm((ne, RW_WIDTH), 0.02),
        'w_in_odd': nrm((no, D_MODEL, E_IN_ODD), D_MODEL ** -0.5),
        'w_out_odd': nrm((no, MIX_WIDTH_ODD, D_MODEL), 0.5 * MIX_WIDTH_ODD ** -0.5),
        'ml_i_bias': -2.0 + nrm((no, ML_HEADS), 0.5),
        'ml_f_bias': uni((no, ML_HEADS), 3.0, 6.0),
        'ml_norm': 1.0 + nrm((no, ML_HEADS * ML_DV), 0.05),
        'mb_conv_w': nrm((no, MB_CONV, MB_XBC), 0.5 * MB_CONV ** -0.5),
        'mb_conv_b': nrm((no, MB_XBC), 0.02),
        'mb_dt_bias': dt0 + jnp.log(-jnp.expm1(-dt0)),
        'mb_A_log': jnp.log(uni((no, MB_HEADS), 1.0, 16.0)),
        'mb_D': 1.0 + nrm((no, MB_HEADS), 0.1),
        'mb_norm': 1.0 + nrm((no, MB_D_INNER), 0.05),
        'ffn_w_up': nrm((DEPTH, D_MODEL, 2 * D_FF), D_MODEL ** -0.5),
        'ffn_w_down': nrm((DEPTH, D_FF, D_MODEL), 0.5 * D_FF ** -0.5),
    }


def reference(x, norm_mix, norm_ffn, norm_final, w_in_even, w_out_even, hg_lb_table, hg_norm, rw_mu, rw_w0,
              rw_w2, rw_a0, rw_a2, rw_g2, rw_k_k, rw_k_a, rw_r_k, rw_ln_w, rw_ln_b, w_in_odd, w_out_odd,
              ml_i_bias, ml_f_bias, ml_norm, mb_conv_w, mb_conv_b, mb_dt_bias, mb_A_log, mb_D, mb_norm,
              ffn_w_up, ffn_w_down):
    h = x
    for layer in range(DEPTH):
        u = rms_norm(h, norm_mix[layer])
        j = layer // 2
        if layer % 2 == 0:
            m = hgrn2_rwkv7_mixer(u, layer, w_in_even[j], w_out_even[j], hg_lb_table, hg_norm[j], rw_mu[j],
                                  rw_w0[j], rw_w2[j], rw_a0[j], rw_a2[j], rw_g2[j], rw_k_k[j], rw_k_a[j],
                                  rw_r_k[j], rw_ln_w[j], rw_ln_b[j])
        else:
            m = mlstm_mamba2_mixer(u, w_in_odd[j], w_out_odd[j], ml_i_bias[j], ml_f_bias[j], ml_norm[j],
                                   mb_conv_w[j], mb_conv_b[j], mb_dt_bias[j], mb_A_log[j], mb_D[j], mb_norm[j])
        h = h + m.astype(h.dtype)
        h = h + swiglu(rms_norm(h, norm_ffn[layer]), ffn_w_up[layer], ffn_w_down[layer]).astype(h.dtype)
    return rms_norm(h, norm_final)
```

```python
import numpy as np
import concourse.bass as bass
import concourse.mybir as mybir
from concourse.alu_op_type import AluOpType as ALU

F32 = mybir.dt.float32
BF16 = mybir.dt.bfloat16
AF = mybir.ActivationFunctionType
AX = mybir.AxisListType


class KB:
    def __init__(self):
        self.nc = bass.Bass("TRN2", target_bir_lowering=False)
        nc = self.nc
        self.eng = {"pe": nc.tensor, "dve": nc.vector, "act": nc.scalar, "pool": nc.gpsimd, "sp": nc.sync}
        self.LIMIT = 8000
        self.epoch = {e: 0 for e in self.eng}
        self.sem = {(e, 0): nc.alloc_semaphore(name="s_" + e + "0") for e in self.eng}
        self.cnt = {e: 0 for e in self.eng}
        self.dsem = {}
        self.dcnt = {}
        self.seen = {e: {} for e in self.eng}
        self.recs = {}
        self.n_ps = 0
        self.out_dma = []
        self.scr_rr = {}

    def sb(self, name, shape, dt=F32):
        return self.nc.alloc_sbuf_tensor(name, list(shape), dt)

    def ps(self, name, shape, dt=F32):
        return self.nc.alloc_psum_tensor(name, list(shape), dt)

    def dram(self, name, shape, dt=F32, kind="ExternalInput"):
        return self.nc.dram_tensor(name, list(shape), dt, kind=kind).ap()

    @staticmethod
    def _region(ap):
        a = ap.ap
        pstep, pcount = a[0]
        off = ap.offset
        if ap.space == "DRAM" or str(ap.space) == "DRAM":
            lo = off
            hi = off + sum((c - 1) * abs(s) for s, c in a) + 1
            return (0, 1, lo, hi)
        if pstep == 0:
            p0, f0 = 0, off
            return (0, 128, f0, f0 + sum((c - 1) * abs(s) for s, c in a[1:]) + 1)
        p0 = off // pstep
        f0 = off % pstep
        hi = f0 + sum((c - 1) * abs(s) for s, c in a[1:]) + 1
        return (p0, p0 + pcount, f0, hi)

    @staticmethod
    def _overlap(r1, r2):
        return r1[0] < r2[1] and r2[0] < r1[1] and r1[2] < r2[3] and r2[2] < r1[3]

    @staticmethod
    def _contains(big, small):
        return big[0] <= small[0] and big[1] >= small[1] and big[2] <= small[2] and big[3] >= small[3]

    def _deps(self, e, reads, writes):
        need = {}
        for ap in reads:
            reg = self._region(ap)
            for (r, sk, isw), c in self.recs.get(ap.tensor.name, {}).items():
                if isw and self._overlap(reg, r):
                    if isinstance(sk, tuple) and sk[0] == e and e == "pe":
                        continue
                    need[sk] = max(need.get(sk, 0), c)
        for ap in writes:
            reg = self._region(ap)
            for (r, sk, isw), c in self.recs.get(ap.tensor.name, {}).items():
                if self._overlap(reg, r):
                    if isinstance(sk, tuple) and sk[0] == e and (e == "pe" or not isw):
                        continue
                    need[sk] = max(need.get(sk, 0), c)
        return need

    def _semof(self, sk):
        return self.sem[sk] if sk in self.sem else self.dsem[sk]

    def _emit_waits(self, e, need):
        for sk, c in need.items():
            if self.seen[e].get(sk, 0) < c:
                self.eng[e].wait_ge(self._semof(sk), c)
                self.seen[e][sk] = c

    def _record(self, sk, c, reads, writes):
        for ap in reads:
            d = self.recs.setdefault(ap.tensor.name, {})
            d[(self._region(ap), sk, False)] = c
        for ap in writes:
            d = self.recs.setdefault(ap.tensor.name, {})
            reg = self._region(ap)
            for k in [k for k in d if self._contains(reg, k[0])]:
                del d[k]
            d[(reg, sk, True)] = c

    def op(self, e, fn, *args, reads=None, writes=None, **kw):
        aps_in = []
        out = kw.get("out", None)
        lst = list(args) + [v for k, v in kw.items() if k != "out"]
        if out is None:
            out = args[0]
            lst = list(args[1:]) + list(kw.values())
        for a in lst:
            if isinstance(a, bass.AP):
                aps_in.append(a)
        if reads is None:
            reads = aps_in
        if writes is None:
            writes = [out]
            if isinstance(kw.get("accum_out", None), bass.AP):
                writes.append(kw["accum_out"])
                reads = [a for a in reads if a is not kw["accum_out"]]
        need = self._deps(e, reads, writes)
        self._emit_waits(e, need)
        if self.cnt[e] >= self.LIMIT:
            self.epoch[e] += 1
            self.cnt[e] = 0
            self.sem[(e, self.epoch[e])] = self.nc.alloc_semaphore(name="s_" + e + str(self.epoch[e]))
        ins = getattr(self.eng[e], fn)(*args, **kw)
        self.cnt[e] += 1
        sk = (e, self.epoch[e])
        ins.then_inc(self.sem[sk], 1)
        self._record(sk, self.cnt[e], reads, writes)
        return ins

    def dma(self, q, out, in_, **kw):
        need = self._deps("dma", [in_], [out])
        self._emit_waits(q, need)
        tn = out.tensor.name
        if str(out.space) == "DRAM":
            if tn.startswith("scr"):
                i = self.scr_rr.get(tn, 0)
                self.scr_rr[tn] = i + 1
                name = "d_%s_r%d" % (tn, i % 8)
            else:
                name = "d_" + tn
        else:
            name = "d_%s_%s" % (tn, "_".join(str(v) for v in self._region(out)))
        if name not in self.dsem:
            self.dsem[name] = self.nc.alloc_semaphore(name=name)
            self.dcnt[name] = 0
        ins = self.eng[q].dma_start(out=out, in_=in_, **kw)
        self.dcnt[name] += 16
        ins.then_inc(self.dsem[name], 16)
        self._record(name, self.dcnt[name], [in_], [out])
        if str(out.space) == "DRAM":
            self.out_dma.append((name, self.dcnt[name]))
        return ins

    def finish(self):
        last = {}
        for sk, c in self.out_dma:
            last[sk] = max(last.get(sk, 0), c)
        for sk, c in last.items():
            self.eng["sp"].wait_ge(self.dsem[sk], c)
        return self.nc


EPS = 1e-6
NT = 1024


def rmsnorm_fm(kb, h_sb, g_sb, uT, ones32, sq, rstd, pss, nt=NT):
    nh = nt // 512
    for c in range(16):
        kb.op("act", "activation", sq[:, c % 2, :], h_sb[:, c, :], AF.Square)
        for j in range(nh):
            kb.op("pe", "matmul", pss[j][:, :], ones32[:, :], sq[:, c % 2, j * 512:(j + 1) * 512],
                  start=(c == 0), stop=(c == 15))
    for j in range(nh):
        kb.op("act", "activation", rstd[:, j * 512:(j + 1) * 512], pss[j][:, :], AF.Sqrt,
              bias=kb.eps_ap, scale=1.0 / 2048.0)
    kb.op("dve", "reciprocal", rstd[:, :], rstd[:, :])
    for c in range(16):
        kb.op("dve", "scalar_tensor_tensor", uT[:, c, :], h_sb[:, c, :], g_sb[:, c:c + 1], rstd[:, :],
              ALU.mult, ALU.mult)


def consts(kb):
    kb.eps_t = kb.sb("eps_t", [128, 1])
    kb.op("pool", "memset", kb.eps_t[:, :], EPS)
    kb.eps_ap = kb.eps_t[:, 0:1]


def build_d1(E):
    kb = KB()
    hT = kb.dram("hT", [2048, NT])
    g = kb.dram("g", [128, 16])
    W = kb.dram("W", [2048, E])
    p = kb.dram("p", [NT, E], kind="ExternalOutput")
    consts(kb)
    h_sb = kb.sb("h_sb", [128, 16, NT])
    uT = kb.sb("uT", [128, 16, NT], BF16)
    g_sb = kb.sb("g_sb", [128, 16])
    ones32 = kb.sb("ones32", [128, 128])
    sq = kb.sb("sq", [128, 2, NT])
    rstd = kb.sb("rstd", [128, NT])
    wb = [kb.sb(f"wb{i}", [128, 16, 512], BF16) for i in range(2)]
    ob = [kb.sb(f"ob{i}", [128, 512]) for i in range(4)]
    pss = [kb.ps(f"ps{i}", [128, 512]) for i in range(8)]
    kb.op("pool", "memset", ones32[:, :], 1.0)
    kb.dma("sp", g_sb[:, :], g[:, :])
    hv = hT.rearrange("(c p) t -> p c t", p=128)
    for c in range(0, 16, 4):
        kb.dma("sp", h_sb[:, c:c + 4, :], hv[:, c:c + 4, :])
    rmsnorm_fm(kb, h_sb, g_sb, uT, ones32, sq, rstd, pss[0:2])
    Wv = W.rearrange("(c p) n -> p c n", p=128)
    nb = (E + 511) // 512
    k = 0
    for b in range(nb):
        n0 = b * 512
        n = min(512, E - n0)
        w = wb[b % 2]
        for c0 in range(0, 16, 4):
            kb.dma("pool", w[:, c0:c0 + 4, 0:n], Wv[:, c0:c0 + 4, n0:n0 + n])
        for m in range(NT // 128):
            pt = pss[2 + k % 6]
            o = ob[k % 4]
            for c in range(16):
                kb.op("pe", "matmul", pt[:, 0:n], uT[:, c, m * 128:(m + 1) * 128], w[:, c, 0:n],
                      start=(c == 0), stop=(c == 15))
            if k % 2 == 0:
                kb.op("dve", "tensor_copy", o[:, 0:n], pt[:, 0:n])
            else:
                kb.op("act", "copy", o[:, 0:n], pt[:, 0:n])
            kb.dma("sp", p[m * 128:(m + 1) * 128, n0:n0 + n], o[:, 0:n])
            k += 1
    return kb.finish()


TT = 512


def rmsnorm_fm2(kb, h_sb, g_sb, out, ones32, sq, rstd, ps, nchunk=16, width=2048.0, eps=EPS, gate=None):
    for c in range(nchunk):
        kb.op("act", "activation", sq[:, c % 2, :], h_sb[:, c, :], AF.Square)
        kb.op("pe", "matmul", ps[:, :], ones32[:, :], sq[:, c % 2, :], start=(c == 0), stop=(c == nchunk - 1))
    kb.op("act", "activation", rstd[:, :], ps[:, :], AF.Sqrt, bias=kb.eps_ap if eps == EPS else eps, scale=1.0 / width)
    kb.op("dve", "reciprocal", rstd[:, :], rstd[:, :])
    for c in range(nchunk):
        if gate is None:
            kb.op("dve", "scalar_tensor_tensor", out[:, c, :], h_sb[:, c, :], g_sb[:, c:c + 1], rstd[:, :],
                  ALU.mult, ALU.mult)
        else:
            kb.op("dve", "scalar_tensor_tensor", sq[:, 0, :], h_sb[:, c, :], g_sb[:, c:c + 1], rstd[:, :],
                  ALU.mult, ALU.mult)
            kb.op("pool", "tensor_tensor", out[:, c, :], sq[:, 0, :], gate[:, c, :], ALU.mult)


def build_b(ntile, odd=False, final=False):
    kb = KB()
    T = ntile * TT
    DFF = 5632
    hT = kb.dram("hT", [2048, T])
    mixT = kb.dram("mixT", [2048, T])
    if odd:
        gateT = kb.dram("gateT", [1024, T])
        gmix = kb.dram("gmix", [128, 16])
    w_out = kb.dram("w_out", [2048, 2048])
    g_ffn = kb.dram("g_ffn", [128, 16])
    w_up = kb.dram("w_up", [2048, 2 * DFF])
    w_dn = kb.dram("w_dn", [DFF, 2048])
    if final:
        g_fin = kb.dram("g_fin", [128, 16])
    outT = kb.dram("outT", [2048, T], kind="ExternalOutput")
    consts(kb)
    h_sb = kb.sb("h_sb", [128, 16, TT])
    uT = kb.sb("uT", [128, 16, TT], BF16)
    act = kb.sb("act", [128, 44, TT], BF16)
    g_sb = kb.sb("g_sb", [128, 16])
    gf_sb = kb.sb("gf_sb", [128, 16])
    gm_sb = kb.sb("gm_sb", [128, 16])
    ones32 = kb.sb("ones32", [128, 128])
    sq = kb.sb("sq", [128, 2, TT])
    rstd = kb.sb("rstd", [128, TT])
    sg = [kb.sb(f"sg{i}", [128, TT]) for i in range(2)]
    NWB = 2 if odd else 3
    wbuf = [kb.sb(f"wb{i}", [128, 11264], BF16) for i in range(NWB)]
    if odd:
        mraw = kb.sb("mraw", [128, 4, TT])
        gate_sb = kb.sb("gate_sb", [128, 2, TT])
    pss = [kb.ps(f"ps{i}", [128, 512]) for i in range(8)]
    kb.op("pool", "memset", ones32[:, :], 1.0)
    kb.dma("sp", g_sb[:, :], g_ffn[:, :])
    if final:
        kb.dma("sp", gf_sb[:, :], g_fin[:, :])
    if odd:
        kb.dma("sp", gm_sb[:, :], gmix[:, :])
    hv = hT.rearrange("(c p) t -> p c t", p=128)
    mv = mixT.rearrange("(c p) t -> p c t", p=128)
    ov = outT.rearrange("(c p) t -> p c t", p=128)
    wov = w_out.rearrange("(c p) n -> p c n", p=128)
    wuv = w_up.rearrange("(c p) n -> p c n", p=128)
    wdv = w_dn.rearrange("(f p) n -> p f n", p=128)
    wk = 0
    pk = 0
    for t in range(ntile):
        ts = slice(t * TT, (t + 1) * TT)
        for c in range(0, 16, 8):
            kb.dma("sp", h_sb[:, c:c + 8, :], hv[:, c:c + 8, ts])
        if not odd:
            for c in range(0, 16, 8):
                kb.dma("pool", uT[:, c:c + 8, :], mv[:, c:c + 8, ts])
        else:
            gv = gateT.rearrange("(c p) t -> p c t", p=128)
            for hd in range(4):
                kb.dma("sp", mraw[:, 0:2, :], mv[:, 2 * hd:2 * hd + 2, ts])
                kb.dma("sp", gate_sb[:, :, :], gv[:, 2 * hd:2 * hd + 2, ts])
                rmsnorm_fm2(kb, mraw[:, 0:2, :], gm_sb[:, 2 * hd:2 * hd + 2],
                            uT[:, 2 * hd:2 * hd + 2, :], ones32, sq, rstd, pss[0], nchunk=2, width=256.0,
                            gate=gate_sb[:, 0:2, :])
            for gp in range(2):
                kb.dma("sp", mraw[:, 0:4, :], mv[:, 8 + 4 * gp:12 + 4 * gp, ts])
                rmsnorm_fm2(kb, mraw[:, 0:4, :], gm_sb[:, 8 + 4 * gp:12 + 4 * gp],
                            uT[:, 8 + 4 * gp:12 + 4 * gp, :], ones32, sq, rstd, pss[0], nchunk=4, width=512.0)
        for ob in range(4):
            w = wbuf[wk % NWB]; wk += 1
            w3 = w[:, 0:8192].rearrange("p (c n) -> p c n", c=16)
            for c0 in range(0, 16, 8):
                kb.dma("pool", w3[:, c0:c0 + 8, :], wov[:, c0:c0 + 8, ob * 512:(ob + 1) * 512])
            for j in range(4):
                oc = ob * 4 + j
                pt = pss[1 + pk % 7]; pk += 1
                for c in range(16):
                    kb.op("pe", "matmul", pt[:, :], w3[:, c, j * 128:(j + 1) * 128], uT[:, c, :],
                          start=(c == 0), stop=(c == 15))
                kb.op("dve", "tensor_tensor", h_sb[:, oc, :], h_sb[:, oc, :], pt[:, :], ALU.add)
        rmsnorm_fm2(kb, h_sb, g_sb, uT, ones32, sq, rstd, pss[0])
        for fb in range(11):
            wg = wbuf[wk % NWB]; wk += 1
            wu = wbuf[wk % NWB]; wk += 1
            wg3 = wg[:, 0:8192].rearrange("p (c n) -> p c n", c=16)
            wu3 = wu[:, 0:8192].rearrange("p (c n) -> p c n", c=16)
            for c0 in range(0, 16, 8):
                kb.dma("pool", wg3[:, c0:c0 + 8, :], wuv[:, c0:c0 + 8, fb * 512:(fb + 1) * 512])
            for c0 in range(0, 16, 8):
                kb.dma("pool", wu3[:, c0:c0 + 8, :], wuv[:, c0:c0 + 8, DFF + fb * 512:DFF + (fb + 1) * 512])
            for j in range(4):
                f = fb * 4 + j
                pg = pss[1 + pk % 7]; pk += 1
                pu = pss[1 + pk % 7]; pk += 1
                for c in range(16):
                    kb.op("pe", "matmul", pg[:, :], wg3[:, c, j * 128:(j + 1) * 128], uT[:, c, :],
                          start=(c == 0), stop=(c == 15))
                for c in range(16):
                    kb.op("pe", "matmul", pu[:, :], wu3[:, c, j * 128:(j + 1) * 128], uT[:, c, :],
                          start=(c == 0), stop=(c == 15))
                s = sg[f % 2]
                kb.op("act", "activation", s[:, :], pg[:, :], AF.Silu)
                kb.op("dve", "tensor_tensor", act[:, f, :], s[:, :], pu[:, :], ALU.mult)
        for ob in range(8):
            w = wbuf[wk % NWB]; wk += 1
            w3 = w[:, 0:11264].rearrange("p (f n) -> p f n", f=44)
            for f0 in range(0, 44, 11):
                kb.dma("pool", w3[:, f0:f0 + 11, :], wdv[:, f0:f0 + 11, ob * 256:(ob + 1) * 256])
            for j in range(2):
                oc = ob * 2 + j
                pt = pss[1 + pk % 7]; pk += 1
                for f in range(44):
                    kb.op("pe", "matmul", pt[:, :], w3[:, f, j * 128:(j + 1) * 128], act[:, f, :],
                          start=(f == 0), stop=(f == 43))
                kb.op("dve", "tensor_tensor", h_sb[:, oc, :], h_sb[:, oc, :], pt[:, :], ALU.add)
        if final:
            rmsnorm_fm2(kb, h_sb, gf_sb, h_sb, ones32, sq, rstd, pss[0])
        for c in range(0, 16, 8):
            kb.dma("sp", ov[:, c:c + 8, ts], h_sb[:, c:c + 8, :])
    return kb.finish()


def build_f(ntile):
    kb = KB()
    T = ntile * TT
    hT = kb.dram("hT", [2048, T])
    g_fin = kb.dram("g_fin", [128, 16])
    outT = kb.dram("outT", [2048, T], kind="ExternalOutput")
    consts(kb)
    h_sb = kb.sb("h_sb", [128, 16, TT])
    o_sb = kb.sb("o_sb", [128, 16, TT])
    gf_sb = kb.sb("gf_sb", [128, 16])
    ones32 = kb.sb("ones32", [128, 128])
    sq = kb.sb("sq", [128, 2, TT])
    rstd = kb.sb("rstd", [128, TT])
    ps = kb.ps("ps0", [128, 512])
    kb.op("pool", "memset", ones32[:, :], 1.0)
    kb.dma("sp", gf_sb[:, :], g_fin[:, :])
    hv = hT.rearrange("(c p) t -> p c t", p=128)
    ov = outT.rearrange("(c p) t -> p c t", p=128)
    for t in range(ntile):
        ts = slice(t * TT, (t + 1) * TT)
        for c in range(0, 16, 8):
            kb.dma("sp", h_sb[:, c:c + 8, :], hv[:, c:c + 8, ts])
        rmsnorm_fm2(kb, h_sb, gf_sb, o_sb, ones32, sq, rstd, ps)
        for c in range(0, 16, 8):
            kb.dma("sp", ov[:, c:c + 8, ts], o_sb[:, c:c + 8, :])
    return kb.finish()


import math

TB = 256
SBK = 16
NTOK = 8192


def norm_block(kb, hv, blk, h_blk, g_sb, uT, ones32, sq, rstd, ps, halo):
    ts = slice(blk * TB, (blk + 1) * TB)
    if blk > 0:
        kb.op("pool", "tensor_copy", uT[:, :, 0:halo], uT[:, :, TB:TB + halo])
    for c in range(0, 16, 8):
        kb.dma("sp", h_blk[:, c:c + 8, :], hv[:, c:c + 8, ts])
    for c in range(16):
        kb.op("act", "activation", sq[:, c % 2, :], h_blk[:, c, :], AF.Square)
        kb.op("pe", "matmul", ps[:, 0:TB], ones32[:, :], sq[:, c % 2, :], start=(c == 0), stop=(c == 15))
    kb.op("act", "activation", rstd[:, :], ps[:, 0:TB], AF.Sqrt, bias=kb.eps_ap, scale=1.0 / 2048.0)
    kb.op("dve", "reciprocal", rstd[:, :], rstd[:, :])
    for c in range(16):
        kb.op("pool" if c % 2 else "dve", "scalar_tensor_tensor" if True else "", uT[:, c, halo:halo + TB],
              h_blk[:, c, :], g_sb[:, c:c + 1], rstd[:, :], ALU.mult, ALU.mult) if c % 2 == 0 else \
            kb.op("dve", "scalar_tensor_tensor", uT[:, c, halo:halo + TB], h_blk[:, c, :], g_sb[:, c:c + 1],
                  rstd[:, :], ALU.mult, ALU.mult)


def proj_fm(kb, ps, Wsb, col0, ncol, uT, off, n=TB):
    for c in range(16):
        kb.op("pe", "matmul", ps[0:ncol, 0:n], Wsb[:, c, col0:col0 + ncol], uT[:, c, off:off + n],
              start=(c == 0), stop=(c == 15))


def proj_tm(kb, ps, Wsb, col0, ncol, uT, off):
    for c in range(16):
        kb.op("pe", "matmul", ps[:, 0:ncol], uT[:, c, off:off + 128], Wsb[:, c, col0:col0 + ncol],
              start=(c == 0), stop=(c == 15))


def lerp_fm(kb, out, ps_cur, ps_prev, mu_col, tmp, np_=128, n=TB):
    kb.op("act", "copy", tmp[0:np_, 0:n], ps_cur[0:np_, 0:n])
    kb.op("dve", "tensor_tensor", out[0:np_, 0:n], ps_prev[0:np_, 0:n], tmp[0:np_, 0:n], ALU.subtract)
    kb.op("dve", "scalar_tensor_tensor", out[0:np_, 0:n], out[0:np_, 0:n], mu_col, tmp[0:np_, 0:n], ALU.mult, ALU.add)


def build_a_even():
    kb = KB()
    NC = 1184
    hT = kb.dram("hT", [2048, NTOK])
    g = kb.dram("g", [128, 16])
    Wc = kb.dram("Wc", [2048, NC])
    vecs = kb.dram("vecs", [1, 512 + 256 + 5 * 128 + 512])
    colsd = kb.dram("cols", [128, 8])
    lora = kb.dram("lora", [128, 128])
    g2c = kb.dram("g2c", [160, 128])
    outT = kb.dram("outT", [256, NTOK], kind="ExternalOutput")
    scr_h = kb.dram("scr_h", [NTOK, 384], kind="Internal")
    scr_r = kb.dram("scr_r", [2, NTOK, 320], kind="Internal")
    consts(kb)
    eps2 = kb.sb("eps2", [128, 1]); kb.op("pool", "memset", eps2[:, :], 64e-5)
    halo = 1
    h_blk = kb.sb("h_blk", [128, 16, TB])
    uT = kb.sb("uT", [128, 16, TB + halo], BF16)
    g_sb = kb.sb("g_sb", [128, 16])
    ones32 = kb.sb("ones32", [128, 128])
    blk64 = kb.sb("blk64", [128, 128])
    ident = kb.sb("ident", [128, 128])
    hsel = kb.sb("hsel", [2, 128])
    sq = kb.sb("sq", [128, 2, TB])
    rstd = kb.sb("rstd", [128, TB])
    Wsb = kb.sb("Wsb", [128, 16, NC], BF16)
    vrep = kb.sb("vrep", [128, 512 + 256 + 640 + 512])
    cols = kb.sb("cols_sb", [128, 8])
    lora_sb = kb.sb("lora_sb", [128, 128])
    g2a = kb.sb("g2a", [128, 128]); g2b = kb.sb("g2b", [32, 128])
    lb = kb.sb("lb", [128, 128]); oml = kb.sb("oml", [128, 128]); esum = kb.sb("esum", [128, 128])
    pss = [kb.ps(f"ps{i}", [128, 512]) for i in range(8)]
    kb.op("pool", "memset", ones32[:, :], 1.0)
    kb.op("pool", "memset", blk64[:, :], 0.0)
    kb.op("pool", "memset", blk64[0:64, 0:64], 1.0)
    kb.op("pool", "memset", blk64[64:128, 64:128], 1.0)
    kb.op("pool", "memset", ident[:, :], 1.0)
    kb.op("pool", "affine_select", ident[:, :], ident[:, :], [[-1, 128]], ALU.is_equal, 0.0, base=0, channel_multiplier=1)
    kb.op("pool", "memset", hsel[:, :], 1.0)
    kb.op("pool", "affine_select", hsel[:, :], hsel[:, :], [[1, 128]], ALU.is_ge, 0.0, base=0, channel_multiplier=-64)
    kb.op("pool", "affine_select", hsel[:, :], hsel[:, :], [[-1, 128]], ALU.is_ge, 0.0, base=63, channel_multiplier=64)
    kb.dma("sp", g_sb[:, :], g[:, :])
    kb.dma("sp", cols[:, :], colsd[:, :])
    kb.dma("sp", lora_sb[:, :], lora[:, :])
    kb.dma("sp", g2a[:, :], g2c[0:128, :]); kb.dma("sp", g2b[:, :], g2c[128:160, :])
    kb.dma("sp", vrep[:, :], vecs[0:1, :].partition_broadcast(128))
    Wv = Wc.rearrange("(c p) n -> p c n", p=128)
    for c0 in range(0, 16, 4):
        kb.dma("pool", Wsb[:, c0:c0 + 4, :], Wv[:, c0:c0 + 4, :])
    kb.op("act", "activation", vrep[:, 0:512], vrep[:, 0:512], AF.Exp)
    kb.op("dve", "tensor_tensor", esum[:, :], vrep[:, 0:128], vrep[:, 128:256], ALU.add)
    kb.op("dve", "tensor_tensor", esum[:, :], esum[:, :], vrep[:, 256:384], ALU.add)
    kb.op("dve", "tensor_tensor", esum[:, :], esum[:, :], vrep[:, 384:512], ALU.add)
    kb.op("dve", "reciprocal", esum[:, :], esum[:, :])
    kb.op("dve", "tensor_tensor", vrep[:, 0:512], vrep[:, 0:512], vrep[:, 1408:1920], ALU.mult)
    kb.op("dve", "tensor_tensor", lb[:, :], vrep[:, 0:128], vrep[:, 128:256], ALU.add)
    kb.op("dve", "tensor_tensor", lb[:, :], lb[:, :], vrep[:, 256:384], ALU.add)
    kb.op("dve", "tensor_tensor", lb[:, :], lb[:, :], vrep[:, 384:512], ALU.add)
    kb.op("dve", "tensor_tensor", lb[:, :], lb[:, :], esum[:, :], ALU.mult)
    kb.op("dve", "tensor_scalar", oml[:, :], lb[:, :], -1.0, 1.0, ALU.mult, ALU.add)
    MU = vrep[:, 512:768]; W0 = vrep[:, 768:896]; A0 = vrep[:, 896:1024]; KK = vrep[:, 1024:1152]
    KA = vrep[:, 1152:1280]; RK = vrep[:, 1280:1408]
    stH = [kb.sb(f"stH{i}", [128, 384]) for i in range(2)]
    stR = [kb.sb(f"stR{i}", [128, 2, 320]) for i in range(2)]
    t1 = kb.sb("t1", [128, 256]); t2 = kb.sb("t2", [128, 256]); t3 = kb.sb("t3", [128, 128]); t4 = kb.sb("t4", [128, 128])
    aa = kb.sb("aa", [128, 128]); kkt = kb.sb("kkt", [128, 128]); ss = kb.sb("ss", [128, 2]); junk = kb.sb("junk", [128, 128])
    bs = kb.sb("bs", [128, 2]); bsT = kb.sb("bsT", [2, TB])
    fm = {n: kb.sb("fm_" + n, [128, TB]) for n in ["iT", "sgT", "vT", "wa", "th", "gl0", "gl1", "gate", "bfm", "tmp", "oH", "oR", "cen", "y"]}
    sH = kb.sb("sH", [128, 128]); sR = kb.sb("sR", [128, 64]); sa = kb.sb("sa", [128, 1])
    bH = [kb.sb(f"bH{i}", [128, SBK, 384]) for i in range(2)]
    bR = [kb.sb(f"bR{i}", [128, SBK, 320]) for i in range(2)]
    kb.op("pool", "memset", sH[:, :], 0.0); kb.op("pool", "memset", sR[:, :], 0.0)
    kb.op("pool", "memset", uT[:, :, 0:halo], 0.0)
    hv = hT.rearrange("(c p) t -> p c t", p=128)
    sk = 0
    for blk in range(NTOK // TB):
        T0 = blk * TB
        norm_block(kb, hv, blk, h_blk, g_sb, uT, ones32, sq, rstd, pss[0], halo)
        for st in range(TB // 128):
            o1 = halo + st * 128
            tok0 = T0 + st * 128
            sth = stH[st % 2]; str_ = stR[st % 2]
            pc = pss[1]; pp = pss[2]
            proj_tm(kb, pc, Wsb, 0, 512, uT, o1)
            proj_tm(kb, pp, Wsb, 256, 256, uT, o1 - 1)
            kb.op("act", "activation", sth[:, 256:384], pc[:, 0:128], AF.Silu)
            kb.op("act", "activation", t3[:, :], pc[:, 128:256], AF.Sigmoid)
            kb.op("dve", "tensor_tensor", t3[:, :], t3[:, :], oml[:, :], ALU.mult)
            kb.op("dve", "tensor_tensor", sth[:, 0:128], t3[:, :], lb[:, :], ALU.add)
            kb.op("dve", "tensor_tensor", sth[:, 128:256], oml[:, :], t3[:, :], ALU.subtract)
            kb.dma("sp", scr_h[tok0:tok0 + 128, :], sth[:, :])
            kb.op("act", "copy", t1[:, :], pc[:, 256:512])
            kb.op("dve", "tensor_tensor", t2[:, :], pp[:, 0:256], t1[:, :], ALU.subtract)
            kb.op("dve", "tensor_tensor", t2[:, :], t2[:, :], MU, ALU.mult)
            kb.op("dve", "tensor_tensor", t1[:, :], t1[:, :], t2[:, :], ALU.add)
            R_ = t1[:, 0:128]; K_ = t1[:, 128:256]
            pc2 = pss[3]; pp2 = pss[4]
            proj_fm(kb, pc2, Wsb, 896, 128, uT, o1, 128)
            proj_fm(kb, pp2, Wsb, 896, 128, uT, o1 - 1, 128)
            lerp_fm(kb, fm["wa"], pc2, pp2, cols[:, 2:3], fm["tmp"], 128, 128)
            kb.op("act", "activation", fm["th"][0:64, 0:128], fm["wa"][0:64, 0:128], AF.Tanh)
            pw = pss[5]; pa = pss[6]
            kb.op("pe", "matmul", pw[:, 0:128], fm["th"][0:64, 0:128], lora_sb[0:64, :], start=True, stop=True)
            kb.op("pe", "matmul", pa[:, 0:128], fm["wa"][64:128, 0:128], lora_sb[64:128, :], start=True, stop=True)
            str3 = str_[:, :, :]
            def slot(i):
                return str_[:, :, i * 64:(i + 1) * 64]
            def v3(ap):
                return ap.rearrange("p (h k) -> p h k", h=2)
            kb.op("dve", "tensor_tensor", t3[:, :], pw[:, 0:128], W0, ALU.add)
            kb.op("act", "activation", t3[:, :], t3[:, :], AF.Sigmoid)
            kb.op("act", "activation", slot(3), v3(t3[:, :]), AF.Exp, scale=-math.exp(-0.5))
            kb.op("dve", "tensor_tensor", aa[:, :], pa[:, 0:128], A0, ALU.add)
            kb.op("act", "activation", aa[:, :], aa[:, :], AF.Sigmoid)
            kb.op("dve", "tensor_tensor", kkt[:, :], K_, KK, ALU.mult)
            for h in range(2):
                kb.op("dve", "scalar_tensor_tensor", junk[:, 0:64], kkt[:, h * 64:(h + 1) * 64], 1.0,
                      kkt[:, h * 64:(h + 1) * 64], ALU.mult, ALU.mult, accum_out=ss[:, h:h + 1])
            kb.op("act", "activation", ss[:, :], ss[:, :], AF.Sqrt)
            kb.op("dve", "tensor_scalar", ss[:, :], ss[:, :], 1e-12, None, ALU.max)
            kb.op("dve", "reciprocal", ss[:, :], ss[:, :])
            kb.op("dve", "tensor_scalar", ss[:, :], ss[:, :], -1.0, None, ALU.mult)
            for h in range(2):
                kb.op("dve", "tensor_scalar", str_[:, h, 0:64], kkt[:, h * 64:(h + 1) * 64], ss[:, h:h + 1], None, ALU.mult)
            kb.op("dve", "scalar_tensor_tensor", slot(1), slot(0), -1.0, v3(aa[:, :]), ALU.mult, ALU.mult)
            kb.op("dve", "scalar_tensor_tensor", t4[:, :], aa[:, :], -1.0, KA, ALU.add, ALU.mult)
            kb.op("dve", "scalar_tensor_tensor", t4[:, :], t4[:, :], 1.0, K_, ALU.add, ALU.mult)
            kb.op("act", "copy", slot(2), v3(t4[:, :]))
            kb.op("act", "copy", slot(4), v3(R_))
            for h in range(2):
                kb.dma("sp", scr_r[h, tok0:tok0 + 128, :], str_[:, h, :])
            kb.op("dve", "tensor_tensor", t3[:, :], R_, RK, ALU.mult)
            for h in range(2):
                kb.op("dve", "scalar_tensor_tensor", junk[:, 0:64], t3[:, h * 64:(h + 1) * 64], 1.0,
                      t4[:, h * 64:(h + 1) * 64], ALU.mult, ALU.mult, accum_out=bs[:, h:h + 1])
            pT = pss[7]
            kb.op("pe", "transpose", pT[0:2, 0:128], bs[:, :], ident[:, :])
            kb.op("act", "copy", bsT[:, st * 128:(st + 1) * 128], pT[0:2, 0:128])
        o0 = halo
        p1 = pss[1]; p2 = pss[2]
        proj_fm(kb, p1, Wsb, 512, 128, uT, o0)
        kb.op("act", "copy", fm["iT"][:, :], p1[:, 0:TB])
        proj_fm(kb, p2, Wsb, 640, 128, uT, o0)
        kb.op("act", "activation", fm["sgT"][:, :], p2[:, 0:TB], AF.Sigmoid)
        p3 = pss[3]; p4 = pss[4]
        proj_fm(kb, p3, Wsb, 768, 128, uT, o0); proj_fm(kb, p4, Wsb, 768, 128, uT, o0 - 1)
        lerp_fm(kb, fm["vT"], p3, p4, cols[:, 1:2], fm["tmp"])
        p5 = pss[5]; p6 = pss[6]
        proj_fm(kb, p5, Wsb, 1024, 128, uT, o0); proj_fm(kb, p6, Wsb, 1024, 128, uT, o0 - 1)
        lerp_fm(kb, fm["gl0"], p5, p6, cols[:, 3:4], fm["tmp"])
        kb.op("act", "activation", fm["gl0"][:, :], fm["gl0"][:, :], AF.Sigmoid)
        proj_fm(kb, p3, Wsb, 1152, 32, uT, o0); proj_fm(kb, p4, Wsb, 1152, 32, uT, o0 - 1)
        lerp_fm(kb, fm["gl1"], p3, p4, cols[0:32, 4:5], fm["tmp"], 32)
        kb.op("act", "activation", fm["gl1"][0:32, :], fm["gl1"][0:32, :], AF.Sigmoid)
        kb.op("pe", "matmul", p5[:, 0:TB], g2a[:, :], fm["gl0"][:, :], start=True, stop=False)
        kb.op("pe", "matmul", p5[:, 0:TB], g2b[:, :], fm["gl1"][0:32, :], start=False, stop=True)
        kb.op("act", "copy", fm["gate"][:, :], p5[:, 0:TB])
        kb.op("pe", "matmul", p6[:, 0:TB], hsel[:, :], bsT[:, :], start=True, stop=True)
        kb.op("dve", "tensor_tensor", fm["bfm"][:, :], p6[:, 0:TB], fm["vT"][:, :], ALU.mult)
        for sb in range(TB // SBK):
            tok0 = T0 + sb * SBK
            bh = bH[sk % 2]; br = bR[sk % 2]; sk += 1
            kb.dma("sp", bh[:, :, :], scr_h[tok0:tok0 + SBK, :].partition_broadcast(128))
            for h in range(2):
                kb.dma("sp", br[h * 64:(h + 1) * 64, :, :], scr_r[h, tok0:tok0 + SBK, :].partition_broadcast(64))
            for i in range(SBK):
                t = sb * SBK + i
                kb.op("dve", "tensor_tensor", sH[:, :], sH[:, :], bh[:, i, 0:128], ALU.mult)
                kb.op("dve", "scalar_tensor_tensor", sH[:, :], bh[:, i, 128:256], fm["iT"][:, t:t + 1], sH[:, :], ALU.mult, ALU.add)
                kb.op("dve", "scalar_tensor_tensor", junk[:, :], sH[:, :], 1.0, bh[:, i, 256:384], ALU.mult, ALU.mult,
                      accum_out=fm["oH"][:, t:t + 1])
                kb.op("dve", "scalar_tensor_tensor", junk[:, 0:64], sR[:, :], 1.0, br[:, i, 0:64], ALU.mult, ALU.mult,
                      accum_out=sa[:, 0:1])
                kb.op("dve", "tensor_tensor", sR[:, :], sR[:, :], br[:, i, 192:256], ALU.mult)
                kb.op("dve", "scalar_tensor_tensor", sR[:, :], br[:, i, 64:128], sa[:, 0:1], sR[:, :], ALU.mult, ALU.add)
                kb.op("dve", "scalar_tensor_tensor", sR[:, :], br[:, i, 128:192], fm["vT"][:, t:t + 1], sR[:, :], ALU.mult, ALU.add)
                kb.op("dve", "scalar_tensor_tensor", junk[:, 0:64], sR[:, :], 1.0, br[:, i, 256:320], ALU.mult, ALU.mult,
                      accum_out=fm["oR"][:, t:t + 1])
        ts = slice(T0, T0 + TB)
        pe1 = pss[1]
        kb.op("act", "activation", sq[:, 0, :], fm["oH"][:, :], AF.Square)
        kb.op("pe", "matmul", pe1[:, 0:TB], ones32[:, :], sq[:, 0, :], start=True, stop=True)
        kb.op("act", "activation", rstd[:, :], pe1[:, 0:TB], AF.Sqrt, bias=kb.eps_ap, scale=1.0 / 128.0)
        kb.op("dve", "reciprocal", rstd[:, :], rstd[:, :])
        kb.op("dve", "scalar_tensor_tensor", fm["y"][:, :], fm["oH"][:, :], cols[:, 0:1], rstd[:, :], ALU.mult, ALU.mult)
        kb.op("dve", "tensor_tensor", fm["y"][:, :], fm["y"][:, :], fm["sgT"][:, :], ALU.mult)
        kb.dma("sp", outT[0:128, ts], fm["y"][:, :])
        pe2 = pss[2]; pe3 = pss[3]
        kb.op("pe", "matmul", pe2[:, 0:TB], blk64[:, :], fm["oR"][:, :], start=True, stop=True)
        kb.op("dve", "scalar_tensor_tensor", fm["cen"][:, :], pe2[:, 0:TB], -1.0 / 64.0, fm["oR"][:, :], ALU.mult, ALU.add)
        kb.op("act", "activation", sq[:, 1, :], fm["cen"][:, :], AF.Square)
        kb.op("pe", "matmul", pe3[:, 0:TB], blk64[:, :], sq[:, 1, :], start=True, stop=True)
        kb.op("act", "activation", rstd[:, :], pe3[:, 0:TB], AF.Sqrt, bias=eps2[:, 0:1], scale=1.0 / 64.0)
        kb.op("dve", "reciprocal", rstd[:, :], rstd[:, :])
        kb.op("dve", "scalar_tensor_tensor", fm["cen"][:, :], fm["cen"][:, :], cols[:, 5:6], rstd[:, :], ALU.mult, ALU.mult)
        kb.op("dve", "scalar_tensor_tensor", fm["cen"][:, :], fm["cen"][:, :], cols[:, 6:7], fm["bfm"][:, :], ALU.add, ALU.add)
        kb.op("dve", "tensor_tensor", fm["cen"][:, :], fm["cen"][:, :], fm["gate"][:, :], ALU.mult)
        kb.dma("sp", outT[128:256, ts], fm["cen"][:, :])
    return kb.finish()


DBG = set()


def build_a_odd():
    kb = KB()
    NC = 1408
    halo = 3
    hT = kb.dram("hT", [2048, NTOK])
    g = kb.dram("g", [128, 16])
    Wc = kb.dram("Wc", [2048, NC])
    colsd = kb.dram("cols", [128, 24])
    outT = kb.dram("outT", [384, NTOK], kind="ExternalOutput")
    scr_m = kb.dram("scr_m", [NTOK, 256], kind="Internal")
    scr_s = kb.dram("scr_s", [NTOK, 256], kind="Internal")
    consts(kb)
    one_c = kb.sb("one_c", [128, 1]); kb.op("pool", "memset", one_c[:, :], 1.0)
    h_blk = kb.sb("h_blk", [128, 16, TB])
    uT = kb.sb("uT", [128, 16, TB + halo], BF16)
    g_sb = kb.sb("g_sb", [128, 16])
    ones32 = kb.sb("ones32", [128, 128])
    ident = kb.sb("ident", [128, 128])
    sq = kb.sb("sq", [128, 2, TB])
    rstd = kb.sb("rstd", [128, TB])
    Wsb = kb.sb("Wsb", [128, 16, NC], BF16)
    cols = kb.sb("cols_sb", [128, 24])
    ibs = kb.sb("ibs", [128, 2]); negA = kb.sb("negA", [128, 1])
    pss = [kb.ps(f"ps{i}", [128, 512]) for i in range(8)]
    kb.op("pool", "memset", ones32[:, :], 1.0)
    kb.op("pool", "memset", ident[:, :], 1.0)
    kb.op("pool", "affine_select", ident[:, :], ident[:, :], [[-1, 128]], ALU.is_equal, 0.0, base=0, channel_multiplier=1)
    kb.dma("sp", g_sb[:, :], g[:, :])
    kb.dma("sp", cols[:, :], colsd[:, :])
    Wv = Wc.rearrange("(c p) n -> p c n", p=128)
    for c0 in range(0, 16, 4):
        kb.dma("pool", Wsb[:, c0:c0 + 4, :], Wv[:, c0:c0 + 4, :])
    kb.op("dve", "tensor_scalar", ibs[:, :], cols[:, 0:2], 1.0 / 15.0, None, ALU.mult)
    kb.op("act", "activation", negA[:, :], cols[:, 18:19], AF.Exp)
    kb.op("dve", "tensor_scalar", negA[:, :], negA[:, :], -1.0, None, ALU.mult)
    stM = [kb.sb(f"stM{i}", [128, 256]) for i in range(2)]
    stS = [kb.sb(f"stS{i}", [128, 256]) for i in range(2)]
    fm = {n: kb.sb("fm_" + n, [128, TB]) for n in ["vT", "ei", "f", "vi", "sgo", "zs", "xc", "Bc", "Cc", "dt", "dA", "dtx",
                                                    "acc", "num", "den", "y", "tmp", "o1", "o2"]}
    junk = kb.sb("junk", [128, 128])
    sM = kb.sb("sM", [128, 256]); sS = kb.sb("sS", [128, 128])
    bM = [kb.sb(f"bM{i}", [128, SBK, 256]) for i in range(2)]
    bS = [kb.sb(f"bS{i}", [128, SBK, 256]) for i in range(2)]
    kb.op("pool", "memset", sM[:, :], 0.0); kb.op("pool", "memset", sS[:, :], 0.0)
    kb.op("pool", "memset", uT[:, :, 0:halo], 0.0)
    hv = hT.rearrange("(c p) t -> p c t", p=128)
    sk = 0

    def conv(dst, col0, cw0, cb):
        ps4 = [pss[1], pss[2], pss[3], pss[4]]
        for j in range(4):
            proj_fm(kb, ps4[j], Wsb, col0, 128, uT, halo - 3 + j)
        kb.op("dve", "tensor_scalar", fm["acc"][:, :], ps4[0][:, 0:TB], cols[:, cw0:cw0 + 1], None, ALU.mult)
        for j in range(1, 4):
            kb.op("dve", "scalar_tensor_tensor", fm["acc"][:, :], ps4[j][:, 0:TB], cols[:, cw0 + j:cw0 + j + 1],
                  fm["acc"][:, :], ALU.mult, ALU.add)
        kb.op("act", "activation", dst[:, :], fm["acc"][:, :], AF.Silu, bias=cols[:, cb:cb + 1])

    for blk in range(NTOK // TB):
        T0 = blk * TB
        ts = slice(T0, T0 + TB)
        norm_block(kb, hv, blk, h_blk, g_sb, uT, ones32, sq, rstd, pss[0], halo)
        for st in (range(TB // 128) if "tm" not in DBG else []):
            o1 = halo + st * 128
            tok0 = T0 + st * 128
            stm = stM[st % 2]
            pc = pss[5]
            proj_tm(kb, pc, Wsb, 0, 256, uT, o1)
            kb.op("dve", "tensor_scalar", stm[:, 0:128], pc[:, 0:128], 128.0 ** -0.5, None, ALU.mult)
            kb.op("dve", "tensor_copy", stm[:, 128:256], pc[:, 128:256])
            kb.dma("sp", scr_m[tok0:tok0 + 128, :], stm[:, :])
        o0 = halo
        p = pss[6]
        proj_fm(kb, p, Wsb, 256, 128, uT, o0)
        kb.op("act", "copy", fm["vT"][:, :], p[:, 0:TB])
        p = pss[7]
        proj_fm(kb, p, Wsb, 384, 128, uT, o0)
        kb.op("act", "activation", fm["ei"][:, :], p[:, 0:TB], AF.Tanh, bias=ibs[:, 0:1], scale=1.0 / 15.0)
        kb.op("act", "activation", fm["ei"][:, :], fm["ei"][:, :], AF.Exp, scale=15.0)
        p = pss[6]
        proj_fm(kb, p, Wsb, 512, 128, uT, o0)
        kb.op("act", "activation", fm["f"][:, :], p[:, 0:TB], AF.Tanh, bias=ibs[:, 1:2], scale=1.0 / 15.0)
        kb.op("act", "activation", fm["f"][:, :], fm["f"][:, :], AF.Sigmoid, scale=15.0)
        p = pss[7]
        proj_fm(kb, p, Wsb, 640, 128, uT, o0)
        kb.op("act", "activation", fm["sgo"][:, :], p[:, 0:TB], AF.Sigmoid)
        kb.dma("sp", outT[128:256, ts], fm["sgo"][:, :])
        kb.op("dve", "tensor_tensor", fm["vi"][:, :], fm["vT"][:, :], fm["ei"][:, :], ALU.mult)
        p = pss[6]
        proj_fm(kb, p, Wsb, 768, 128, uT, o0)
        kb.op("act", "activation", fm["zs"][:, :], p[:, 0:TB], AF.Silu)
        conv(fm["xc"], 896, 2, 6)
        conv(fm["Bc"], 1024, 7, 11)
        conv(fm["Cc"], 1152, 12, 16)
        p = pss[7]
        proj_fm(kb, p, Wsb, 1280, 128, uT, o0)
        kb.op("act", "activation", fm["dt"][:, :], p[:, 0:TB], AF.Exp, bias=cols[:, 17:18])
        kb.op("act", "activation", fm["dt"][:, :], fm["dt"][:, :], AF.Ln, bias=one_c[:, 0:1])
        kb.op("act", "activation", fm["dA"][:, :], fm["dt"][:, :], AF.Exp, scale=negA[:, 0:1])
        kb.op("dve", "tensor_tensor", fm["dtx"][:, :], fm["dt"][:, :], fm["xc"][:, :], ALU.mult)
        for st in (range(TB // 128) if "tr" not in DBG else []):
            tok0 = T0 + st * 128
            sts = stS[st % 2]
            pT = pss[5]
            kb.op("pe", "transpose", pT[:, 0:128], fm["Bc"][:, st * 128:(st + 1) * 128], ident[:, :])
            kb.op("pe", "transpose", pT[:, 128:256], fm["Cc"][:, st * 128:(st + 1) * 128], ident[:, :])
            kb.op("act", "copy", sts[:, :], pT[:, 0:256])
            kb.dma("sp", scr_s[tok0:tok0 + 128, :], sts[:, :])
        for sb in (range(TB // SBK) if "rec" not in DBG else []):
            tok0 = T0 + sb * SBK
            bm = bM[sk % 2]; bs_ = bS[sk % 2]; sk += 1
            kb.dma("sp", bm[:, :, :], scr_m[tok0:tok0 + SBK, :].partition_broadcast(128))
            kb.dma("sp", bs_[:, :, :], scr_s[tok0:tok0 + SBK, :].partition_broadcast(128))
            for i in range(SBK):
                t = sb * SBK + i
                kb.op("dve", "tensor_scalar", sM[:, :], sM[:, :], fm["f"][:, t:t + 1], None, ALU.mult)
                kb.op("dve", "scalar_tensor_tensor", sM[:, 0:128], bm[:, i, 128:256], fm["vi"][:, t:t + 1], sM[:, 0:128], ALU.mult, ALU.add)
                kb.op("dve", "scalar_tensor_tensor", sM[:, 128:256], bm[:, i, 128:256], fm["ei"][:, t:t + 1], sM[:, 128:256], ALU.mult, ALU.add)
                kb.op("dve", "scalar_tensor_tensor", junk[:, :], sM[:, 0:128], 1.0, bm[:, i, 0:128], ALU.mult, ALU.mult,
                      accum_out=fm["num"][:, t:t + 1])
                kb.op("dve", "scalar_tensor_tensor", junk[:, :], sM[:, 128:256], 1.0, bm[:, i, 0:128], ALU.mult, ALU.mult,
                      accum_out=fm["den"][:, t:t + 1])
                kb.op("dve", "tensor_scalar", sS[:, :], sS[:, :], fm["dA"][:, t:t + 1], None, ALU.mult)
                kb.op("dve", "scalar_tensor_tensor", sS[:, :], bs_[:, i, 0:128], fm["dtx"][:, t:t + 1], sS[:, :], ALU.mult, ALU.add)
                kb.op("dve", "scalar_tensor_tensor", junk[:, :], sS[:, :], 1.0, bs_[:, i, 128:256], ALU.mult, ALU.mult,
                      accum_out=fm["y"][:, t:t + 1])
        kb.op("dve", "scalar_tensor_tensor", fm["tmp"][:, :], fm["den"][:, :], -1.0, fm["den"][:, :], ALU.mult, ALU.max)
        kb.op("dve", "tensor_scalar", fm["den"][:, :], fm["tmp"][:, :], 1.0, None, ALU.max)
        kb.op("dve", "reciprocal", fm["den"][:, :], fm["den"][:, :])
        kb.op("dve", "tensor_tensor", fm["o1"][:, :], fm["num"][:, :], fm["den"][:, :], ALU.mult)
        kb.dma("sp", outT[0:128, ts], fm["o1"][:, :])
        kb.op("dve", "scalar_tensor_tensor", fm["o2"][:, :], fm["xc"][:, :], cols[:, 19:20], fm["y"][:, :], ALU.mult, ALU.add)
        kb.op("dve", "tensor_tensor", fm["o2"][:, :], fm["o2"][:, :], fm["zs"][:, :], ALU.mult)
        kb.dma("sp", outT[256:384, ts], fm["o2"][:, :])
    return kb.finish()


def lay16(v):
    return np.ascontiguousarray(v.reshape(16, 128).T)

def prep_a_even(d, j, layer, hT):
    w_in = d['w_in_even'][j]; mu = d['rw_mu'][j]
    maps = []
    for c in range(8):
        s = slice(c * 128, (c + 1) * 128)
        cs = lambda base: w_in[:, base + c * 128: base + (c + 1) * 128]
        Wc = np.concatenate([cs(0), cs(1024), cs(4096), cs(4096 + 1024), cs(2048), cs(3072), cs(4096 + 2048),
                             w_in[:, 4096 + 3072:4096 + 3136], w_in[:, 4096 + 3136:4096 + 3200],
                             w_in[:, 4096 + 3200:4096 + 3328], w_in[:, 4096 + 3328:4096 + 3360]], axis=1)
        vecs = np.concatenate([d['hg_lb_table'][:, s].reshape(-1), mu[0:1024][s], mu[1024:2048][s],
                               d['rw_w0'][j][s], d['rw_a0'][j][s], d['rw_k_k'][j][s], d['rw_k_a'][j][s],
                               d['rw_r_k'][j].reshape(-1)[s],
                               np.repeat((np.arange(4) >= 1) & (np.arange(4) <= layer), 128).astype(np.float32)])[None, :]
        cols = np.zeros((128, 8), np.float32)
        cols[:, 0] = d['hg_norm'][j][s]; cols[:, 1] = mu[2048:3072][s]
        cols[:, 2] = mu[3072:3200]; cols[:, 3] = mu[3200:3328]; cols[0:32, 4] = mu[3328:3360]
        cols[:, 5] = d['rw_ln_w'][j][s]; cols[:, 6] = d['rw_ln_b'][j][s]
        lora = np.concatenate([d['rw_w2'][j][:, s], d['rw_a2'][j][:, s]], axis=0)
        maps.append({"hT": hT, "g": lay16(d['norm_mix'][layer]), "Wc": np.ascontiguousarray(Wc),
                     "vecs": np.ascontiguousarray(vecs.astype(np.float32)), "cols": cols,
                     "lora": np.ascontiguousarray(lora), "g2c": np.ascontiguousarray(d['rw_g2'][j][:, s])})
    return maps

def gather_a_even(results):
    mixT = np.zeros((2048, 8192), np.float32)
    for c, r in enumerate(results):
        o = r["outT"]
        mixT[c * 128:(c + 1) * 128] = o[0:128]
        mixT[1024 + c * 128:1024 + (c + 1) * 128] = o[128:256]
    return mixT


def prep_a_odd(d, j, layer, hT):
    w = d['w_in_odd'][j]
    MB = 3080
    cw = d['mb_conv_w'][j]; cbv = d['mb_conv_b'][j]
    maps = []
    for c in range(8):
        hd, half, gi = c // 2, c % 2, c // 4
        rep = lambda col: np.repeat(w[:, col:col + 1], 128, axis=1)
        vcol = 1024 + hd * 256 + half * 128
        ocol = 2056 + hd * 256 + half * 128
        dtrep = np.concatenate([np.repeat(w[:, MB + 2560 + 2 * c:MB + 2560 + 2 * c + 1], 64, axis=1),
                                np.repeat(w[:, MB + 2560 + 2 * c + 1:MB + 2560 + 2 * c + 2], 64, axis=1)], axis=1)
        xch = slice(c * 128, (c + 1) * 128)
        bch = slice(1024 + gi * 128, 1024 + (gi + 1) * 128)
        cch = slice(1280 + gi * 128, 1280 + (gi + 1) * 128)
        Wc = np.concatenate([w[:, hd * 128:(hd + 1) * 128], w[:, 512 + hd * 128:512 + (hd + 1) * 128],
                             w[:, vcol:vcol + 128], rep(2048 + hd), rep(2052 + hd), w[:, ocol:ocol + 128],
                             w[:, MB + c * 128:MB + (c + 1) * 128],
                             w[:, MB + 1024 + xch.start:MB + 1024 + xch.stop],
                             w[:, MB + 1024 + bch.start:MB + 1024 + bch.stop],
                             w[:, MB + 1024 + cch.start:MB + 1024 + cch.stop], dtrep], axis=1)
        cols = np.zeros((128, 24), np.float32)
        cols[:, 0] = d['ml_i_bias'][j][hd]; cols[:, 1] = d['ml_f_bias'][j][hd]
        cols[:, 2:6] = cw[:, xch].T; cols[:, 6] = cbv[xch]
        cols[:, 7:11] = cw[:, bch].T; cols[:, 11] = cbv[bch]
        cols[:, 12:16] = cw[:, cch].T; cols[:, 16] = cbv[cch]
        hl = np.repeat(np.array([2 * c, 2 * c + 1]), 64)
        cols[:, 17] = d['mb_dt_bias'][j][hl]; cols[:, 18] = d['mb_A_log'][j][hl]; cols[:, 19] = d['mb_D'][j][hl]
        maps.append({"hT": hT, "g": lay16(d['norm_mix'][layer]), "Wc": np.ascontiguousarray(Wc), "cols": cols})
    return maps


def gather_a_odd(results):
    mixT = np.zeros((2048, 8192), np.float32)
    gateT = np.zeros((1024, 8192), np.float32)
    for c, r in enumerate(results):
        o = r["outT"]
        mixT[c * 128:(c + 1) * 128] = o[0:128]
        gateT[c * 128:(c + 1) * 128] = o[128:256]
        mixT[1024 + c * 128:1024 + (c + 1) * 128] = o[256:384]
    return mixT, gateT


from concourse.bass_utils import run_bass_kernel_spmd

B_NTILE = 8
B_NCORE = 16 // B_NTILE


def kernel(**inp):
    d = {k: np.asarray(v) for k, v in inp.items()}
    hT = np.ascontiguousarray(d['x'][0].T)
    T = B_NTILE * TT
    for layer in range(4):
        j = layer // 2
        odd = layer % 2 == 1
        if not odd:
            res = run_bass_kernel_spmd(build_a_even(), prep_a_even(d, j, layer, hT), core_ids=list(range(8)))
            mixT = gather_a_even(res.results)
            w_out = d['w_out_even'][j]
        else:
            res = run_bass_kernel_spmd(build_a_odd(), prep_a_odd(d, j, layer, hT), core_ids=list(range(8)))
            mixT, gateT = gather_a_odd(res.results)
            w_out = d['w_out_odd'][j]
        del res
        maps = []
        for c in range(B_NCORE):
            m = {"hT": np.ascontiguousarray(hT[:, c * T:(c + 1) * T]),
                 "mixT": np.ascontiguousarray(mixT[:, c * T:(c + 1) * T]),
                 "w_out": w_out, "g_ffn": lay16(d['norm_ffn'][layer]),
                 "w_up": d['ffn_w_up'][layer], "w_dn": d['ffn_w_down'][layer]}
            if odd:
                m["gateT"] = np.ascontiguousarray(gateT[:, c * T:(c + 1) * T])
                m["gmix"] = lay16(np.concatenate([d['ml_norm'][j], d['mb_norm'][j]]))
            maps.append(m)
        res = run_bass_kernel_spmd(build_b(B_NTILE, odd=odd, final=False), maps, core_ids=list(range(B_NCORE)))
        hT = np.ascontiguousarray(np.concatenate([r["outT"] for r in res.results], axis=1))
        del res
    TF = 2 * TT
    maps = [{"hT": np.ascontiguousarray(hT[:, c * TF:(c + 1) * TF]), "g_fin": lay16(d['norm_final'])} for c in range(8)]
    res = run_bass_kernel_spmd(build_f(2), maps, core_ids=list(range(8)))
    yT = np.concatenate([r["outT"] for r in res.results], axis=1)
    return np.ascontiguousarray(yT.T)[None].astype(np.float32)
```

```python
import numpy as np
import concourse.bass as bass
import concourse.mybir as mybir
from concourse.alu_op_type import AluOpType as ALU

F32 = mybir.dt.float32
BF16 = mybir.dt.bfloat16
AF = mybir.ActivationFunctionType
AX = mybir.AxisListType


class KB:
    def __init__(self):
        self.nc = bass.Bass("TRN2", target_bir_lowering=False)
        nc = self.nc
        self.eng = {"pe": nc.tensor, "dve": nc.vector, "act": nc.scalar, "pool": nc.gpsimd, "sp": nc.sync}
        self.LIMIT = 8000
        self.epoch = {e: 0 for e in self.eng}
        self.sem = {(e, 0): nc.alloc_semaphore(name="s_" + e + "0") for e in self.eng}
        self.cnt = {e: 0 for e in self.eng}
        self.dsem = {}
        self.dcnt = {}
        self.seen = {e: {} for e in self.eng}
        self.recs = {}
        self.n_ps = 0
        self.out_dma = []
        self.scr_rr = {}

    def sb(self, name, shape, dt=F32):
        return self.nc.alloc_sbuf_tensor(name, list(shape), dt)

    def ps(self, name, shape, dt=F32):
        return self.nc.alloc_psum_tensor(name, list(shape), dt)

    def dram(self, name, shape, dt=F32, kind="ExternalInput"):
        return self.nc.dram_tensor(name, list(shape), dt, kind=kind).ap()

    @staticmethod
    def _region(ap):
        a = ap.ap
        pstep, pcount = a[0]
        off = ap.offset
        if ap.space == "DRAM" or str(ap.space) == "DRAM":
            lo = off
            hi = off + sum((c - 1) * abs(s) for s, c in a) + 1
            return (0, 1, lo, hi)
        if pstep == 0:
            p0, f0 = 0, off
            return (0, 128, f0, f0 + sum((c - 1) * abs(s) for s, c in a[1:]) + 1)
        p0 = off // pstep
        f0 = off % pstep
        hi = f0 + sum((c - 1) * abs(s) for s, c in a[1:]) + 1
        return (p0, p0 + pcount, f0, hi)

    @staticmethod
    def _overlap(r1, r2):
        return r1[0] < r2[1] and r2[0] < r1[1] and r1[2] < r2[3] and r2[2] < r1[3]

    @staticmethod
    def _contains(big, small):
        return big[0] <= small[0] and big[1] >= small[1] and big[2] <= small[2] and big[3] >= small[3]

    def _deps(self, e, reads, writes):
        need = {}
        for ap in reads:
            reg = self._region(ap)
            for (r, sk, isw), c in self.recs.get(ap.tensor.name, {}).items():
                if isw and self._overlap(reg, r):
                    if isinstance(sk, tuple) and sk[0] == e and e == "pe":
                        continue
                    need[sk] = max(need.get(sk, 0), c)
        for ap in writes:
            reg = self._region(ap)
            for (r, sk, isw), c in self.recs.get(ap.tensor.name, {}).items():
                if self._overlap(reg, r):
                    if isinstance(sk, tuple) and sk[0] == e and (e == "pe" or not isw):
                        continue
                    need[sk] = max(need.get(sk, 0), c)
        return need

    def _semof(self, sk):
        return self.sem[sk] if sk in self.sem else self.dsem[sk]

    def _emit_waits(self, e, need):
        for sk, c in need.items():
            if self.seen[e].get(sk, 0) < c:
                self.eng[e].wait_ge(self._semof(sk), c)
                self.seen[e][sk] = c

    def _record(self, sk, c, reads, writes):
        for ap in reads:
            d = self.recs.setdefault(ap.tensor.name, {})
            d[(self._region(ap), sk, False)] = c
        for ap in writes:
            d = self.recs.setdefault(ap.tensor.name, {})
            reg = self._region(ap)
            for k in [k for k in d if self._contains(reg, k[0])]:
                del d[k]
            d[(reg, sk, True)] = c

    def op(self, e, fn, *args, reads=None, writes=None, **kw):
        aps_in = []
        out = kw.get("out", None)
        lst = list(args) + [v for k, v in kw.items() if k != "out"]
        if out is None:
            out = args[0]
            lst = list(args[1:]) + list(kw.values())
        for a in lst:
            if isinstance(a, bass.AP):
                aps_in.append(a)
        if reads is None:
            reads = aps_in
        if writes is None:
            writes = [out]
            if isinstance(kw.get("accum_out", None), bass.AP):
                writes.append(kw["accum_out"])
                reads = [a for a in reads if a is not kw["accum_out"]]
        need = self._deps(e, reads, writes)
        self._emit_waits(e, need)
        if self.cnt[e] >= self.LIMIT:
            self.epoch[e] += 1
            self.cnt[e] = 0
            self.sem[(e, self.epoch[e])] = self.nc.alloc_semaphore(name="s_" + e + str(self.epoch[e]))
        ins = getattr(self.eng[e], fn)(*args, **kw)
        self.cnt[e] += 1
        sk = (e, self.epoch[e])
        ins.then_inc(self.sem[sk], 1)
        self._record(sk, self.cnt[e], reads, writes)
        return ins

    def dma(self, q, out, in_, **kw):
        need = self._deps("dma", [in_], [out])
        self._emit_waits(q, need)
        tn = out.tensor.name
        if str(out.space) == "DRAM":
            if tn.startswith("scr"):
                i = self.scr_rr.get(tn, 0)
                self.scr_rr[tn] = i + 1
                name = "d_%s_r%d" % (tn, i % 8)
            else:
                name = "d_" + tn
        else:
            name = "d_%s_%s" % (tn, "_".join(str(v) for v in self._region(out)))
        if name not in self.dsem:
            self.dsem[name] = self.nc.alloc_semaphore(name=name)
            self.dcnt[name] = 0
        ins = self.eng[q].dma_start(out=out, in_=in_, **kw)
        self.dcnt[name] += 16
        ins.then_inc(self.dsem[name], 16)
        self._record(name, self.dcnt[name], [in_], [out])
        if str(out.space) == "DRAM":
            self.out_dma.append((name, self.dcnt[name]))
        return ins

    def finish(self):
        last = {}
        for sk, c in self.out_dma:
            last[sk] = max(last.get(sk, 0), c)
        for sk, c in last.items():
            self.eng["sp"].wait_ge(self.dsem[sk], c)
        return self.nc


EPS = 1e-6
NT = 1024


def rmsnorm_fm(kb, h_sb, g_sb, uT, ones32, sq, rstd, pss, nt=NT):
    nh = nt // 512
    for c in range(16):
        kb.op("act", "activation", sq[:, c % 2, :], h_sb[:, c, :], AF.Square)
        for j in range(nh):
            kb.op("pe", "matmul", pss[j][:, :], ones32[:, :], sq[:, c % 2, j * 512:(j + 1) * 512],
                  start=(c == 0), stop=(c == 15))
    for j in range(nh):
        kb.op("act", "activation", rstd[:, j * 512:(j + 1) * 512], pss[j][:, :], AF.Sqrt,
              bias=kb.eps_ap, scale=1.0 / 2048.0)
    kb.op("dve", "reciprocal", rstd[:, :], rstd[:, :])
    for c in range(16):
        kb.op("dve", "scalar_tensor_tensor", uT[:, c, :], h_sb[:, c, :], g_sb[:, c:c + 1], rstd[:, :],
              ALU.mult, ALU.mult)


def consts(kb):
    kb.eps_t = kb.sb("eps_t", [128, 1])
    kb.op("pool", "memset", kb.eps_t[:, :], EPS)
    kb.eps_ap = kb.eps_t[:, 0:1]


def build_d1(E):
    kb = KB()
    hT = kb.dram("hT", [2048, NT])
    g = kb.dram("g", [128, 16])
    W = kb.dram("W", [2048, E])
    p = kb.dram("p", [NT, E], kind="ExternalOutput")
    consts(kb)
    h_sb = kb.sb("h_sb", [128, 16, NT])
    uT = kb.sb("uT", [128, 16, NT], BF16)
    g_sb = kb.sb("g_sb", [128, 16])
    ones32 = kb.sb("ones32", [128, 128])
    sq = kb.sb("sq", [128, 2, NT])
    rstd = kb.sb("rstd", [128, NT])
    wb = [kb.sb(f"wb{i}", [128, 16, 512], BF16) for i in range(2)]
    ob = [kb.sb(f"ob{i}", [128, 512]) for i in range(4)]
    pss = [kb.ps(f"ps{i}", [128, 512]) for i in range(8)]
    kb.op("pool", "memset", ones32[:, :], 1.0)
    kb.dma("sp", g_sb[:, :], g[:, :])
    hv = hT.rearrange("(c p) t -> p c t", p=128)
    for c in range(0, 16, 4):
        kb.dma("sp", h_sb[:, c:c + 4, :], hv[:, c:c + 4, :])
    rmsnorm_fm(kb, h_sb, g_sb, uT, ones32, sq, rstd, pss[0:2])
    Wv = W.rearrange("(c p) n -> p c n", p=128)
    nb = (E + 511) // 512
    k = 0
    for b in range(nb):
        n0 = b * 512
        n = min(512, E - n0)
        w = wb[b % 2]
        for c0 in range(0, 16, 4):
            kb.dma("pool", w[:, c0:c0 + 4, 0:n], Wv[:, c0:c0 + 4, n0:n0 + n])
        for m in range(NT // 128):
            pt = pss[2 + k % 6]
            o = ob[k % 4]
            for c in range(16):
                kb.op("pe", "matmul", pt[:, 0:n], uT[:, c, m * 128:(m + 1) * 128], w[:, c, 0:n],
                      start=(c == 0), stop=(c == 15))
            if k % 2 == 0:
                kb.op("dve", "tensor_copy", o[:, 0:n], pt[:, 0:n])
            else:
                kb.op("act", "copy", o[:, 0:n], pt[:, 0:n])
            kb.dma("sp", p[m * 128:(m + 1) * 128, n0:n0 + n], o[:, 0:n])
            k += 1
    return kb.finish()


TT = 512


def rmsnorm_fm2(kb, h_sb, g_sb, out, ones32, sq, rstd, ps, nchunk=16, width=2048.0, eps=EPS, gate=None):
    for c in range(nchunk):
        kb.op("act", "activation", sq[:, c % 2, :], h_sb[:, c, :], AF.Square)
        kb.op("pe", "matmul", ps[:, :], ones32[:, :], sq[:, c % 2, :], start=(c == 0), stop=(c == nchunk - 1))
    kb.op("act", "activation", rstd[:, :], ps[:, :], AF.Sqrt, bias=kb.eps_ap if eps == EPS else eps, scale=1.0 / width)
    kb.op("dve", "reciprocal", rstd[:, :], rstd[:, :])
    for c in range(nchunk):
        if gate is None:
            kb.op("dve", "scalar_tensor_tensor", out[:, c, :], h_sb[:, c, :], g_sb[:, c:c + 1], rstd[:, :],
                  ALU.mult, ALU.mult)
        else:
            kb.op("dve", "scalar_tensor_tensor", sq[:, 0, :], h_sb[:, c, :], g_sb[:, c:c + 1], rstd[:, :],
                  ALU.mult, ALU.mult)
            kb.op("pool", "tensor_tensor", out[:, c, :], sq[:, 0, :], gate[:, c, :], ALU.mult)


def build_b(ntile, odd=False, final=False):
    kb = KB()
    T = ntile * TT
    DFF = 5632
    hT = kb.dram("hT", [2048, T])
    mixT = kb.dram("mixT", [2048, T])
    if odd:
        gateT = kb.dram("gateT", [1024, T])
        gmix = kb.dram("gmix", [128, 16])
    w_out = kb.dram("w_out", [2048, 2048])
    g_ffn = kb.dram("g_ffn", [128, 16])
    w_up = kb.dram("w_up", [2048, 2 * DFF])
    w_dn = kb.dram("w_dn", [DFF, 2048])
    if final:
        g_fin = kb.dram("g_fin", [128, 16])
    outT = kb.dram("outT", [2048, T], kind="ExternalOutput")
    consts(kb)
    h_sb = kb.sb("h_sb", [128, 16, TT])
    uT = kb.sb("uT", [128, 16, TT], BF16)
    act = kb.sb("act", [128, 44, TT], BF16)
    g_sb = kb.sb("g_sb", [128, 16])
    gf_sb = kb.sb("gf_sb", [128, 16])
    gm_sb = kb.sb("gm_sb", [128, 16])
    ones32 = kb.sb("ones32", [128, 128])
    sq = kb.sb("sq", [128, 2, TT])
    rstd = kb.sb("rstd", [128, TT])
    sg = [kb.sb(f"sg{i}", [128, TT]) for i in range(2)]
    NWB = 2 if odd else 3
    wbuf = [kb.sb(f"wb{i}", [128, 11264], BF16) for i in range(NWB)]
    if odd:
        mraw = kb.sb("mraw", [128, 4, TT])
        gate_sb = kb.sb("gate_sb", [128, 2, TT])
    pss = [kb.ps(f"ps{i}", [128, 512]) for i in range(8)]
    kb.op("pool", "memset", ones32[:, :], 1.0)
    kb.dma("sp", g_sb[:, :], g_ffn[:, :])
    if final:
        kb.dma("sp", gf_sb[:, :], g_fin[:, :])
    if odd:
        kb.dma("sp", gm_sb[:, :], gmix[:, :])
    hv = hT.rearrange("(c p) t -> p c t", p=128)
    mv = mixT.rearrange("(c p) t -> p c t", p=128)
    ov = outT.rearrange("(c p) t -> p c t", p=128)
    wov = w_out.rearrange("(c p) n -> p c n", p=128)
    wuv = w_up.rearrange("(c p) n -> p c n", p=128)
    wdv = w_dn.rearrange("(f p) n -> p f n", p=128)
    wk = 0
    pk = 0
    for t in range(ntile):
        ts = slice(t * TT, (t + 1) * TT)
        for c in range(0, 16, 8):
            kb.dma("sp", h_sb[:, c:c + 8, :], hv[:, c:c + 8, ts])
        if not odd:
            for c in range(0, 16, 8):
                kb.dma("pool", uT[:, c:c + 8, :], mv[:, c:c + 8, ts])
        else:
            gv = gateT.rearrange("(c p) t -> p c t", p=128)
            for hd in range(4):
                kb.dma("sp", mraw[:, 0:2, :], mv[:, 2 * hd:2 * hd + 2, ts])
                kb.dma("sp", gate_sb[:, :, :], gv[:, 2 * hd:2 * hd + 2, ts])
                rmsnorm_fm2(kb, mraw[:, 0:2, :], gm_sb[:, 2 * hd:2 * hd + 2],
                            uT[:, 2 * hd:2 * hd + 2, :], ones32, sq, rstd, pss[0], nchunk=2, width=256.0,
                            gate=gate_sb[:, 0:2, :])
            for gp in range(2):
                kb.dma("sp", mraw[:, 0:4, :], mv[:, 8 + 4 * gp:12 + 4 * gp, ts])
                rmsnorm_fm2(kb, mraw[:, 0:4, :], gm_sb[:, 8 + 4 * gp:12 + 4 * gp],
                            uT[:, 8 + 4 * gp:12 + 4 * gp, :], ones32, sq, rstd, pss[0], nchunk=4, width=512.0)
        for ob in range(4):
            w = wbuf[wk % NWB]; wk += 1
            w3 = w[:, 0:8192].rearrange("p (c n) -> p c n", c=16)
            for c0 in range(0, 16, 8):
                kb.dma("pool", w3[:, c0:c0 + 8, :], wov[:, c0:c0 + 8, ob * 512:(ob + 1) * 512])
            for j in range(4):
                oc = ob * 4 + j
                pt = pss[1 + pk % 7]; pk += 1
                for c in range(16):
                    kb.op("pe", "matmul", pt[:, :], w3[:, c, j * 128:(j + 1) * 128], uT[:, c, :],
                          start=(c == 0), stop=(c == 15))
                kb.op("dve", "tensor_tensor", h_sb[:, oc, :], h_sb[:, oc, :], pt[:, :], ALU.add)
        rmsnorm_fm2(kb, h_sb, g_sb, uT, ones32, sq, rstd, pss[0])
        for fb in range(11):
            wg = wbuf[wk % NWB]; wk += 1
            wu = wbuf[wk % NWB]; wk += 1
            wg3 = wg[:, 0:8192].rearrange("p (c n) -> p c n", c=16)
            wu3 = wu[:, 0:8192].rearrange("p (c n) -> p c n", c=16)
            for c0 in range(0, 16, 8):
                kb.dma("pool", wg3[:, c0:c0 + 8, :], wuv[:, c0:c0 + 8, fb * 512:(fb + 1) * 512])
            for c0 in range(0, 16, 8):
                kb.dma("pool", wu3[:, c0:c0 + 8, :], wuv[:, c0:c0 + 8, DFF + fb * 512:DFF + (fb + 1) * 512])
            for j in range(4):
                f = fb * 4 + j
                pg = pss[1 + pk % 7]; pk += 1
                pu = pss[1 + pk % 7]; pk += 1
                for c in range(16):
                    kb.op("pe", "matmul", pg[:, :], wg3[:, c, j * 128:(j + 1) * 128], uT[:, c, :],
                          start=(c == 0), stop=(c == 15))
                for c in range(16):
                    kb.op("pe", "matmul", pu[:, :], wu3[:, c, j * 128:(j + 1) * 128], uT[:, c, :],
                          start=(c == 0), stop=(c == 15))
                s = sg[f % 2]
                kb.op("act", "activation", s[:, :], pg[:, :], AF.Silu)
                kb.op("dve", "tensor_tensor", act[:, f, :], s[:, :], pu[:, :], ALU.mult)
        for ob in range(8):
            w = wbuf[wk % NWB]; wk += 1
            w3 = w[:, 0:11264].rearrange("p (f n) -> p f n", f=44)
            for f0 in range(0, 44, 11):
                kb.dma("pool", w3[:, f0:f0 + 11, :], wdv[:, f0:f0 + 11, ob * 256:(ob + 1) * 256])
            for j in range(2):
                oc = ob * 2 + j
                pt = pss[1 + pk % 7]; pk += 1
                for f in range(44):
                    kb.op("pe", "matmul", pt[:, :], w3[:, f, j * 128:(j + 1) * 128], act[:, f, :],
                          start=(f == 0), stop=(f == 43))
                kb.op("dve", "tensor_tensor", h_sb[:, oc, :], h_sb[:, oc, :], pt[:, :], ALU.add)
        if final:
            rmsnorm_fm2(kb, h_sb, gf_sb, h_sb, ones32, sq, rstd, pss[0])
        for c in range(0, 16, 8):
            kb.dma("sp", ov[:, c:c + 8, ts], h_sb[:, c:c + 8, :])
    return kb.finish()


def build_f(ntile):
    kb = KB()
    T = ntile * TT
    hT = kb.dram("hT", [2048, T])
    g_fin = kb.dram("g_fin", [128, 16])
    outT = kb.dram("outT", [2048, T], kind="ExternalOutput")
    consts(kb)
    h_sb = kb.sb("h_sb", [128, 16, TT])
    o_sb = kb.sb("o_sb", [128, 16, TT])
    gf_sb = kb.sb("gf_sb", [128, 16])
    ones32 = kb.sb("ones32", [128, 128])
    sq = kb.sb("sq", [128, 2, TT])
    rstd = kb.sb("rstd", [128, TT])
    ps = kb.ps("ps0", [128, 512])
    kb.op("pool", "memset", ones32[:, :], 1.0)
    kb.dma("sp", gf_sb[:, :], g_fin[:, :])
    hv = hT.rearrange("(c p) t -> p c t", p=128)
    ov = outT.rearrange("(c p) t -> p c t", p=128)
    for t in range(ntile):
        ts = slice(t * TT, (t + 1) * TT)
        for c in range(0, 16, 8):
            kb.dma("sp", h_sb[:, c:c + 8, :], hv[:, c:c + 8, ts])
        rmsnorm_fm2(kb, h_sb, gf_sb, o_sb, ones32, sq, rstd, ps)
        for c in range(0, 16, 8):
            kb.dma("sp", ov[:, c:c + 8, ts], o_sb[:, c:c + 8, :])
    return kb.finish()


import math

TB = 256
SBK = 16
NTOK = 8192


def norm_block(kb, hv, blk, h_blk, g_sb, uT, ones32, sq, rstd, ps, halo):
    ts = slice(blk * TB, (blk + 1) * TB)
    if blk > 0:
        kb.op("pool", "tensor_copy", uT[:, :, 0:halo], uT[:, :, TB:TB + halo])
    for c in range(0, 16, 8):
        kb.dma("sp", h_blk[:, c:c + 8, :], hv[:, c:c + 8, ts])
    for c in range(16):
        kb.op("act", "activation", sq[:, c % 2, :], h_blk[:, c, :], AF.Square)
        kb.op("pe", "matmul", ps[:, 0:TB], ones32[:, :], sq[:, c % 2, :], start=(c == 0), stop=(c == 15))
    kb.op("act", "activation", rstd[:, :], ps[:, 0:TB], AF.Sqrt, bias=kb.eps_ap, scale=1.0 / 2048.0)
    kb.op("dve", "reciprocal", rstd[:, :], rstd[:, :])
    for c in range(16):
        kb.op("pool" if c % 2 else "dve", "scalar_tensor_tensor" if True else "", uT[:, c, halo:halo + TB],
              h_blk[:, c, :], g_sb[:, c:c + 1], rstd[:, :], ALU.mult, ALU.mult) if c % 2 == 0 else \
            kb.op("dve", "scalar_tensor_tensor", uT[:, c, halo:halo + TB], h_blk[:, c, :], g_sb[:, c:c + 1],
                  rstd[:, :], ALU.mult, ALU.mult)


def proj_fm(kb, ps, Wsb, col0, ncol, uT, off, n=TB):
    for c in range(16):
        kb.op("pe", "matmul", ps[0:ncol, 0:n], Wsb[:, c, col0:col0 + ncol], uT[:, c, off:off + n],
              start=(c == 0), stop=(c == 15))


def proj_tm(kb, ps, Wsb, col0, ncol, uT, off):
    for c in range(16):
        kb.op("pe", "matmul", ps[:, 0:ncol], uT[:, c, off:off + 128], Wsb[:, c, col0:col0 + ncol],
              start=(c == 0), stop=(c == 15))


def lerp_fm(kb, out, ps_cur, ps_prev, mu_col, tmp, np_=128, n=TB):
    kb.op("act", "copy", tmp[0:np_, 0:n], ps_cur[0:np_, 0:n])
    kb.op("dve", "tensor_tensor", out[0:np_, 0:n], ps_prev[0:np_, 0:n], tmp[0:np_, 0:n], ALU.subtract)
    kb.op("dve", "scalar_tensor_tensor", out[0:np_, 0:n], out[0:np_, 0:n], mu_col, tmp[0:np_, 0:n], ALU.mult, ALU.add)


def build_a_even():
    kb = KB()
    NC = 1184
    hT = kb.dram("hT", [2048, NTOK])
    g = kb.dram("g", [128, 16])
    Wc = kb.dram("Wc", [2048, NC])
    vecs = kb.dram("vecs", [1, 512 + 256 + 5 * 128 + 512])
    colsd = kb.dram("cols", [128, 8])
    lora = kb.dram("lora", [128, 128])
    g2c = kb.dram("g2c", [160, 128])
    outT = kb.dram("outT", [256, NTOK], kind="ExternalOutput")
    scr_h = kb.dram("scr_h", [NTOK, 384], kind="Internal")
    scr_r = kb.dram("scr_r", [2, NTOK, 320], kind="Internal")
    consts(kb)
    eps2 = kb.sb("eps2", [128, 1]); kb.op("pool", "memset", eps2[:, :], 64e-5)
    halo = 1
    h_blk = kb.sb("h_blk", [128, 16, TB])
    uT = kb.sb("uT", [128, 16, TB + halo], BF16)
    g_sb = kb.sb("g_sb", [128, 16])
    ones32 = kb.sb("ones32", [128, 128])
    blk64 = kb.sb("blk64", [128, 128])
    ident = kb.sb("ident", [128, 128])
    hsel = kb.sb("hsel", [2, 128])
    sq = kb.sb("sq", [128, 2, TB])
    rstd = kb.sb("rstd", [128, TB])
    Wsb = kb.sb("Wsb", [128, 16, NC], BF16)
    vrep = kb.sb("vrep", [128, 512 + 256 + 640 + 512])
    cols = kb.sb("cols_sb", [128, 8])
    lora_sb = kb.sb("lora_sb", [128, 128])
    g2a = kb.sb("g2a", [128, 128]); g2b = kb.sb("g2b", [32, 128])
    lb = kb.sb("lb", [128, 128]); oml = kb.sb("oml", [128, 128]); esum = kb.sb("esum", [128, 128])
    pss = [kb.ps(f"ps{i}", [128, 512]) for i in range(8)]
    kb.op("pool", "memset", ones32[:, :], 1.0)
    kb.op("pool", "memset", blk64[:, :], 0.0)
    kb.op("pool", "memset", blk64[0:64, 0:64], 1.0)
    kb.op("pool", "memset", blk64[64:128, 64:128], 1.0)
    kb.op("pool", "memset", ident[:, :], 1.0)
    kb.op("pool", "affine_select", ident[:, :], ident[:, :], [[-1, 128]], ALU.is_equal, 0.0, base=0, channel_multiplier=1)
    kb.op("pool", "memset", hsel[:, :], 1.0)
    kb.op("pool", "affine_select", hsel[:, :], hsel[:, :], [[1, 128]], ALU.is_ge, 0.0, base=0, channel_multiplier=-64)
    kb.op("pool", "affine_select", hsel[:, :], hsel[:, :], [[-1, 128]], ALU.is_ge, 0.0, base=63, channel_multiplier=64)
    kb.dma("sp", g_sb[:, :], g[:, :])
    kb.dma("sp", cols[:, :], colsd[:, :])
    kb.dma("sp", lora_sb[:, :], lora[:, :])
    kb.dma("sp", g2a[:, :], g2c[0:128, :]); kb.dma("sp", g2b[:, :], g2c[128:160, :])
    kb.dma("sp", vrep[:, :], vecs[0:1, :].partition_broadcast(128))
    Wv = Wc.rearrange("(c p) n -> p c n", p=128)
    for c0 in range(0, 16, 4):
        kb.dma("pool", Wsb[:, c0:c0 + 4, :], Wv[:, c0:c0 + 4, :])
    kb.op("act", "activation", vrep[:, 0:512], vrep[:, 0:512], AF.Exp)
    kb.op("dve", "tensor_tensor", esum[:, :], vrep[:, 0:128], vrep[:, 128:256], ALU.add)
    kb.op("dve", "tensor_tensor", esum[:, :], esum[:, :], vrep[:, 256:384], ALU.add)
    kb.op("dve", "tensor_tensor", esum[:, :], esum[:, :], vrep[:, 384:512], ALU.add)
    kb.op("dve", "reciprocal", esum[:, :], esum[:, :])
    kb.op("dve", "tensor_tensor", vrep[:, 0:512], vrep[:, 0:512], vrep[:, 1408:1920], ALU.mult)
    kb.op("dve", "tensor_tensor", lb[:, :], vrep[:, 0:128], vrep[:, 128:256], ALU.add)
    kb.op("dve", "tensor_tensor", lb[:, :], lb[:, :], vrep[:, 256:384], ALU.add)
    kb.op("dve", "tensor_tensor", lb[:, :], lb[:, :], vrep[:, 384:512], ALU.add)
    kb.op("dve", "tensor_tensor", lb[:, :], lb[:, :], esum[:, :], ALU.mult)
    kb.op("dve", "tensor_scalar", oml[:, :], lb[:, :], -1.0, 1.0, ALU.mult, ALU.add)
    MU = vrep[:, 512:768]; W0 = vrep[:, 768:896]; A0 = vrep[:, 896:1024]; KK = vrep[:, 1024:1152]
    KA = vrep[:, 1152:1280]; RK = vrep[:, 1280:1408]
    stH = [kb.sb(f"stH{i}", [128, 384]) for i in range(2)]
    stR = [kb.sb(f"stR{i}", [128, 2, 320]) for i in range(2)]
    t1 = kb.sb("t1", [128, 256]); t2 = kb.sb("t2", [128, 256]); t3 = kb.sb("t3", [128, 128]); t4 = kb.sb("t4", [128, 128])
    aa = kb.sb("aa", [128, 128]); kkt = kb.sb("kkt", [128, 128]); ss = kb.sb("ss", [128, 2]); junk = kb.sb("junk", [128, 128])
    bs = kb.sb("bs", [128, 2]); bsT = kb.sb("bsT", [2, TB])
    fm = {n: kb.sb("fm_" + n, [128, TB]) for n in ["iT", "sgT", "vT", "wa", "th", "gl0", "gl1", "gate", "bfm", "tmp", "oH", "oR", "cen", "y"]}
    sH = kb.sb("sH", [128, 128]); sR = kb.sb("sR", [128, 64]); sa = kb.sb("sa", [128, 1])
    bH = [kb.sb(f"bH{i}", [128, SBK, 384]) for i in range(2)]
    bR = [kb.sb(f"bR{i}", [128, SBK, 320]) for i in range(2)]
    kb.op("pool", "memset", sH[:, :], 0.0); kb.op("pool", "memset", sR[:, :], 0.0)
    kb.op("pool", "memset", uT[:, :, 0:halo], 0.0)
    hv = hT.rearrange("(c p) t -> p c t", p=128)
    sk = 0
    for blk in range(NTOK // TB):
        T0 = blk * TB
        norm_block(kb, hv, blk, h_blk, g_sb, uT, ones32, sq, rstd, pss[0], halo)
        for st in range(TB // 128):
            o1 = halo + st * 128
            tok0 = T0 + st * 128
            sth = stH[st % 2]; str_ = stR[st % 2]
            pc = pss[1]; pp = pss[2]
            proj_tm(kb, pc, Wsb, 0, 512, uT, o1)
            proj_tm(kb, pp, Wsb, 256, 256, uT, o1 - 1)
            kb.op("act", "activation", sth[:, 256:384], pc[:, 0:128], AF.Silu)
            kb.op("act", "activation", t3[:, :], pc[:, 128:256], AF.Sigmoid)
            kb.op("dve", "tensor_tensor", t3[:, :], t3[:, :], oml[:, :], ALU.mult)
            kb.op("dve", "tensor_tensor", sth[:, 0:128], t3[:, :], lb[:, :], ALU.add)
            kb.op("dve", "tensor_tensor", sth[:, 128:256], oml[:, :], t3[:, :], ALU.subtract)
            kb.dma("sp", scr_h[tok0:tok0 + 128, :], sth[:, :])
            kb.op("act", "copy", t1[:, :], pc[:, 256:512])
            kb.op("dve", "tensor_tensor", t2[:, :], pp[:, 0:256], t1[:, :], ALU.subtract)
            kb.op("dve", "tensor_tensor", t2[:, :], t2[:, :], MU, ALU.mult)
            kb.op("dve", "tensor_tensor", t1[:, :], t1[:, :], t2[:, :], ALU.add)
            R_ = t1[:, 0:128]; K_ = t1[:, 128:256]
            pc2 = pss[3]; pp2 = pss[4]
            proj_fm(kb, pc2, Wsb, 896, 128, uT, o1, 128)
            proj_fm(kb, pp2, Wsb, 896, 128, uT, o1 - 1, 128)
            lerp_fm(kb, fm["wa"], pc2, pp2, cols[:, 2:3], fm["tmp"], 128, 128)
            kb.op("act", "activation", fm["th"][0:64, 0:128], fm["wa"][0:64, 0:128], AF.Tanh)
            pw = pss[5]; pa = pss[6]
            kb.op("pe", "matmul", pw[:, 0:128], fm["th"][0:64, 0:128], lora_sb[0:64, :], start=True, stop=True)
            kb.op("pe", "matmul", pa[:, 0:128], fm["wa"][64:128, 0:128], lora_sb[64:128, :], start=True, stop=True)
            str3 = str_[:, :, :]
            def slot(i):
                return str_[:, :, i * 64:(i + 1) * 64]
            def v3(ap):
                return ap.rearrange("p (h k) -> p h k", h=2)
            kb.op("dve", "tensor_tensor", t3[:, :], pw[:, 0:128], W0, ALU.add)
            kb.op("act", "activation", t3[:, :], t3[:, :], AF.Sigmoid)
            kb.op("act", "activation", slot(3), v3(t3[:, :]), AF.Exp, scale=-math.exp(-0.5))
            kb.op("dve", "tensor_tensor", aa[:, :], pa[:, 0:128], A0, ALU.add)
            kb.op("act", "activation", aa[:, :], aa[:, :], AF.Sigmoid)
            kb.op("dve", "tensor_tensor", kkt[:, :], K_, KK, ALU.mult)
            for h in range(2):
                kb.op("dve", "scalar_tensor_tensor", junk[:, 0:64], kkt[:, h * 64:(h + 1) * 64], 1.0,
                      kkt[:, h * 64:(h + 1) * 64], ALU.mult, ALU.mult, accum_out=ss[:, h:h + 1])
            kb.op("act", "activation", ss[:, :], ss[:, :], AF.Sqrt)
            kb.op("dve", "tensor_scalar", ss[:, :], ss[:, :], 1e-12, None, ALU.max)
            kb.op("dve", "reciprocal", ss[:, :], ss[:, :])
            kb.op("dve", "tensor_scalar", ss[:, :], ss[:, :], -1.0, None, ALU.mult)
            for h in range(2):
                kb.op("dve", "tensor_scalar", str_[:, h, 0:64], kkt[:, h * 64:(h + 1) * 64], ss[:, h:h + 1], None, ALU.mult)
            kb.op("dve", "scalar_tensor_tensor", slot(1), slot(0), -1.0, v3(aa[:, :]), ALU.mult, ALU.mult)
            kb.op("dve", "scalar_tensor_tensor", t4[:, :], aa[:, :], -1.0, KA, ALU.add, ALU.mult)
            kb.op("dve", "scalar_tensor_tensor", t4[:, :], t4[:, :], 1.0, K_, ALU.add, ALU.mult)
            kb.op("act", "copy", slot(2), v3(t4[:, :]))
            kb.op("act", "copy", slot(4), v3(R_))
            for h in range(2):
                kb.dma("sp", scr_r[h, tok0:tok0 + 128, :], str_[:, h, :])
            kb.op("dve", "tensor_tensor", t3[:, :], R_, RK, ALU.mult)
            for h in range(2):
                kb.op("dve", "scalar_tensor_tensor", junk[:, 0:64], t3[:, h * 64:(h + 1) * 64], 1.0,
                      t4[:, h * 64:(h + 1) * 64], ALU.mult, ALU.mult, accum_out=bs[:, h:h + 1])
            pT = pss[7]
            kb.op("pe", "transpose", pT[0:2, 0:128], bs[:, :], ident[:, :])
            kb.op("act", "copy", bsT[:, st * 128:(st + 1) * 128], pT[0:2, 0:128])
        o0 = halo
        p1 = pss[1]; p2 = pss[2]
        proj_fm(kb, p1, Wsb, 512, 128, uT, o0)
        kb.op("act", "copy", fm["iT"][:, :], p1[:, 0:TB])
        proj_fm(kb, p2, Wsb, 640, 128, uT, o0)
        kb.op("act", "activation", fm["sgT"][:, :], p2[:, 0:TB], AF.Sigmoid)
        p3 = pss[3]; p4 = pss[4]
        proj_fm(kb, p3, Wsb, 768, 128, uT, o0); proj_fm(kb, p4, Wsb, 768, 128, uT, o0 - 1)
        lerp_fm(kb, fm["vT"], p3, p4, cols[:, 1:2], fm["tmp"])
        p5 = pss[5]; p6 = pss[6]
        proj_fm(kb, p5, Wsb, 1024, 128, uT, o0); proj_fm(kb, p6, Wsb, 1024, 128, uT, o0 - 1)
        lerp_fm(kb, fm["gl0"], p5, p6, cols[:, 3:4], fm["tmp"])
        kb.op("act", "activation", fm["gl0"][:, :], fm["gl0"][:, :], AF.Sigmoid)
        proj_fm(kb, p3, Wsb, 1152, 32, uT, o0); proj_fm(kb, p4, Wsb, 1152, 32, uT, o0 - 1)
        lerp_fm(kb, fm["gl1"], p3, p4, cols[0:32, 4:5], fm["tmp"], 32)
        kb.op("act", "activation", fm["gl1"][0:32, :], fm["gl1"][0:32, :], AF.Sigmoid)
        kb.op("pe", "matmul", p5[:, 0:TB], g2a[:, :], fm["gl0"][:, :], start=True, stop=False)
        kb.op("pe", "matmul", p5[:, 0:TB], g2b[:, :], fm["gl1"][0:32, :], start=False, stop=True)
        kb.op("act", "copy", fm["gate"][:, :], p5[:, 0:TB])
        kb.op("pe", "matmul", p6[:, 0:TB], hsel[:, :], bsT[:, :], start=True, stop=True)
        kb.op("dve", "tensor_tensor", fm["bfm"][:, :], p6[:, 0:TB], fm["vT"][:, :], ALU.mult)
        for sb in range(TB // SBK):
            tok0 = T0 + sb * SBK
            bh = bH[sk % 2]; br = bR[sk % 2]; sk += 1
            kb.dma("sp", bh[:, :, :], scr_h[tok0:tok0 + SBK, :].partition_broadcast(128))
            for h in range(2):
                kb.dma("sp", br[h * 64:(h + 1) * 64, :, :], scr_r[h, tok0:tok0 + SBK, :].partition_broadcast(64))
            for i in range(SBK):
                t = sb * SBK + i
                kb.op("dve", "tensor_tensor", sH[:, :], sH[:, :], bh[:, i, 0:128], ALU.mult)
                kb.op("dve", "scalar_tensor_tensor", sH[:, :], bh[:, i, 128:256], fm["iT"][:, t:t + 1], sH[:, :], ALU.mult, ALU.add)
                kb.op("dve", "scalar_tensor_tensor", junk[:, :], sH[:, :], 1.0, bh[:, i, 256:384], ALU.mult, ALU.mult,
                      accum_out=fm["oH"][:, t:t + 1])
                kb.op("dve", "scalar_tensor_tensor", junk[:, 0:64], sR[:, :], 1.0, br[:, i, 0:64], ALU.mult, ALU.mult,
                      accum_out=sa[:, 0:1])
                kb.op("dve", "tensor_tensor", sR[:, :], sR[:, :], br[:, i, 192:256], ALU.mult)
                kb.op("dve", "scalar_tensor_tensor", sR[:, :], br[:, i, 64:128], sa[:, 0:1], sR[:, :], ALU.mult, ALU.add)
                kb.op("dve", "scalar_tensor_tensor", sR[:, :], br[:, i, 128:192], fm["vT"][:, t:t + 1], sR[:, :], ALU.mult, ALU.add)
                kb.op("dve", "scalar_tensor_tensor", junk[:, 0:64], sR[:, :], 1.0, br[:, i, 256:320], ALU.mult, ALU.mult,
                      accum_out=fm["oR"][:, t:t + 1])
        ts = slice(T0, T0 + TB)
        pe1 = pss[1]
        kb.op("act", "activation", sq[:, 0, :], fm["oH"][:, :], AF.Square)
        kb.op("pe", "matmul", pe1[:, 0:TB], ones32[:, :], sq[:, 0, :], start=True, stop=True)
        kb.op("act", "activation", rstd[:, :], pe1[:, 0:TB], AF.Sqrt, bias=kb.eps_ap, scale=1.0 / 128.0)
        kb.op("dve", "reciprocal", rstd[:, :], rstd[:, :])
        kb.op("dve", "scalar_tensor_tensor", fm["y"][:, :], fm["oH"][:, :], cols[:, 0:1], rstd[:, :], ALU.mult, ALU.mult)
        kb.op("dve", "tensor_tensor", fm["y"][:, :], fm["y"][:, :], fm["sgT"][:, :], ALU.mult)
        kb.dma("sp", outT[0:128, ts], fm["y"][:, :])
        pe2 = pss[2]; pe3 = pss[3]
        kb.op("pe", "matmul", pe2[:, 0:TB], blk64[:, :], fm["oR"][:, :], start=True, stop=True)
        kb.op("dve", "scalar_tensor_tensor", fm["cen"][:, :], pe2[:, 0:TB], -1.0 / 64.0, fm["oR"][:, :], ALU.mult, ALU.add)
        kb.op("act", "activation", sq[:, 1, :], fm["cen"][:, :], AF.Square)
        kb.op("pe", "matmul", pe3[:, 0:TB], blk64[:, :], sq[:, 1, :], start=True, stop=True)
        kb.op("act", "activation", rstd[:, :], pe3[:, 0:TB], AF.Sqrt, bias=eps2[:, 0:1], scale=1.0 / 64.0)
        kb.op("dve", "reciprocal", rstd[:, :], rstd[:, :])
        kb.op("dve", "scalar_tensor_tensor", fm["cen"][:, :], fm["cen"][:, :], cols[:, 5:6], rstd[:, :], ALU.mult, ALU.mult)
        kb.op("dve", "scalar_tensor_tensor", fm["cen"][:, :], fm["cen"][:, :], cols[:, 6:7], fm["bfm"][:, :], ALU.add, ALU.add)
        kb.op("dve", "tensor_tensor", fm["cen"][:, :], fm["cen"][:, :], fm["gate"][:, :], ALU.mult)
        kb.dma("sp", outT[128:256, ts], fm["cen"][:, :])
    return kb.finish()


DBG = set()


def build_a_odd():
    kb = KB()
    NC = 1408
    halo = 3
    hT = kb.dram("hT", [2048, NTOK])
    g = kb.dram("g", [128, 16])
    Wc = kb.dram("Wc", [2048, NC])
    colsd = kb.dram("cols", [128, 24])
    outT = kb.dram("outT", [384, NTOK], kind="ExternalOutput")
    scr_m = kb.dram("scr_m", [NTOK, 256], kind="Internal")
    scr_s = kb.dram("scr_s", [NTOK, 256], kind="Internal")
    consts(kb)
    one_c = kb.sb("one_c", [128, 1]); kb.op("pool", "memset", one_c[:, :], 1.0)
    h_blk = kb.sb("h_blk", [128, 16, TB])
    uT = kb.sb("uT", [128, 16, TB + halo], BF16)
    g_sb = kb.sb("g_sb", [128, 16])
    ones32 = kb.sb("ones32", [128, 128])
    ident = kb.sb("ident", [128, 128])
    sq = kb.sb("sq", [128, 2, TB])
    rstd = kb.sb("rstd", [128, TB])
    Wsb = kb.sb("Wsb", [128, 16, NC], BF16)
    cols = kb.sb("cols_sb", [128, 24])
    ibs = kb.sb("ibs", [128, 2]); negA = kb.sb("negA", [128, 1])
    pss = [kb.ps(f"ps{i}", [128, 512]) for i in range(8)]
    kb.op("pool", "memset", ones32[:, :], 1.0)
    kb.op("pool", "memset", ident[:, :], 1.0)
    kb.op("pool", "affine_select", ident[:, :], ident[:, :], [[-1, 128]], ALU.is_equal, 0.0, base=0, channel_multiplier=1)
    kb.dma("sp", g_sb[:, :], g[:, :])
    kb.dma("sp", cols[:, :], colsd[:, :])
    Wv = Wc.rearrange("(c p) n -> p c n", p=128)
    for c0 in range(0, 16, 4):
        kb.dma("pool", Wsb[:, c0:c0 + 4, :], Wv[:, c0:c0 + 4, :])
    kb.op("dve", "tensor_scalar", ibs[:, :], cols[:, 0:2], 1.0 / 15.0, None, ALU.mult)
    kb.op("act", "activation", negA[:, :], cols[:, 18:19], AF.Exp)
    kb.op("dve", "tensor_scalar", negA[:, :], negA[:, :], -1.0, None, ALU.mult)
    stM = [kb.sb(f"stM{i}", [128, 256]) for i in range(2)]
    stS = [kb.sb(f"stS{i}", [128, 256]) for i in range(2)]
    fm = {n: kb.sb("fm_" + n, [128, TB]) for n in ["vT", "ei", "f", "vi", "sgo", "zs", "xc", "Bc", "Cc", "dt", "dA", "dtx",
                                                    "acc", "num", "den", "y", "tmp", "o1", "o2"]}
    junk = kb.sb("junk", [128, 128])
    sM = kb.sb("sM", [128, 256]); sS = kb.sb("sS", [128, 128])
    bM = [kb.sb(f"bM{i}", [128, SBK, 256]) for i in range(2)]
    bS = [kb.sb(f"bS{i}", [128, SBK, 256]) for i in range(2)]
    kb.op("pool", "memset", sM[:, :], 0.0); kb.op("pool", "memset", sS[:, :], 0.0)
    kb.op("pool", "memset", uT[:, :, 0:halo], 0.0)
    hv = hT.rearrange("(c p) t -> p c t", p=128)
    sk = 0

    def conv(dst, col0, cw0, cb):
        ps4 = [pss[1], pss[2], pss[3], pss[4]]
        for j in range(4):
            proj_fm(kb, ps4[j], Wsb, col0, 128, uT, halo - 3 + j)
        kb.op("dve", "tensor_scalar", fm["acc"][:, :], ps4[0][:, 0:TB], cols[:, cw0:cw0 + 1], None, ALU.mult)
        for j in range(1, 4):
            kb.op("dve", "scalar_tensor_tensor", fm["acc"][:, :], ps4[j][:, 0:TB], cols[:, cw0 + j:cw0 + j + 1],
                  fm["acc"][:, :], ALU.mult, ALU.add)
        kb.op("act", "activation", dst[:, :], fm["acc"][:, :], AF.Silu, bias=cols[:, cb:cb + 1])

    for blk in range(NTOK // TB):
        T0 = blk * TB
        ts = slice(T0, T0 + TB)
        norm_block(kb, hv, blk, h_blk, g_sb, uT, ones32, sq, rstd, pss[0], halo)
        for st in (range(TB // 128) if "tm" not in DBG else []):
            o1 = halo + st * 128
            tok0 = T0 + st * 128
            stm = stM[st % 2]
            pc = pss[5]
            proj_tm(kb, pc, Wsb, 0, 256, uT, o1)
            kb.op("dve", "tensor_scalar", stm[:, 0:128], pc[:, 0:128], 128.0 ** -0.5, None, ALU.mult)
            kb.op("dve", "tensor_copy", stm[:, 128:256], pc[:, 128:256])
            kb.dma("sp", scr_m[tok0:tok0 + 128, :], stm[:, :])
        o0 = halo
        p = pss[6]
        proj_fm(kb, p, Wsb, 256, 128, uT, o0)
        kb.op("act", "copy", fm["vT"][:, :], p[:, 0:TB])
        p = pss[7]
        proj_fm(kb, p, Wsb, 384, 128, uT, o0)
        kb.op("act", "activation", fm["ei"][:, :], p[:, 0:TB], AF.Tanh, bias=ibs[:, 0:1], scale=1.0 / 15.0)
        kb.op("act", "activation", fm["ei"][:, :], fm["ei"][:, :], AF.Exp, scale=15.0)
        p = pss[6]
        proj_fm(kb, p, Wsb, 512, 128, uT, o0)
        kb.op("act", "activation", fm["f"][:, :], p[:, 0:TB], AF.Tanh, bias=ibs[:, 1:2], scale=1.0 / 15.0)
        kb.op("act", "activation", fm["f"][:, :], fm["f"][:, :], AF.Sigmoid, scale=15.0)
        p = pss[7]
        proj_fm(kb, p, Wsb, 640, 128, uT, o0)
        kb.op("act", "activation", fm["sgo"][:, :], p[:, 0:TB], AF.Sigmoid)
        kb.dma("sp", outT[128:256, ts], fm["sgo"][:, :])
        kb.op("dve", "tensor_tensor", fm["vi"][:, :], fm["vT"][:, :], fm["ei"][:, :], ALU.mult)
        p = pss[6]
        proj_fm(kb, p, Wsb, 768, 128, uT, o0)
        kb.op("act", "activation", fm["zs"][:, :], p[:, 0:TB], AF.Silu)
        conv(fm["xc"], 896, 2, 6)
        conv(fm["Bc"], 1024, 7, 11)
        conv(fm["Cc"], 1152, 12, 16)
        p = pss[7]
        proj_fm(kb, p, Wsb, 1280, 128, uT, o0)
        kb.op("act", "activation", fm["dt"][:, :], p[:, 0:TB], AF.Exp, bias=cols[:, 17:18])
        kb.op("act", "activation", fm["dt"][:, :], fm["dt"][:, :], AF.Ln, bias=one_c[:, 0:1])
        kb.op("act", "activation", fm["dA"][:, :], fm["dt"][:, :], AF.Exp, scale=negA[:, 0:1])
        kb.op("dve", "tensor_tensor", fm["dtx"][:, :], fm["dt"][:, :], fm["xc"][:, :], ALU.mult)
        for st in (range(TB // 128) if "tr" not in DBG else []):
            tok0 = T0 + st * 128
            sts = stS[st % 2]
            pT = pss[5]
            kb.op("pe", "transpose", pT[:, 0:128], fm["Bc"][:, st * 128:(st + 1) * 128], ident[:, :])
            kb.op("pe", "transpose", pT[:, 128:256], fm["Cc"][:, st * 128:(st + 1) * 128], ident[:, :])
            kb.op("act", "copy", sts[:, :], pT[:, 0:256])
            kb.dma("sp", scr_s[tok0:tok0 + 128, :], sts[:, :])
        for sb in (range(TB // SBK) if "rec" not in DBG else []):
            tok0 = T0 + sb * SBK
            bm = bM[sk % 2]; bs_ = bS[sk % 2]; sk += 1
            kb.dma("sp", bm[:, :, :], scr_m[tok0:tok0 + SBK, :].partition_broadcast(128))
            kb.dma("sp", bs_[:, :, :], scr_s[tok0:tok0 + SBK, :].partition_broadcast(128))
            for i in range(SBK):
                t = sb * SBK + i
                kb.op("dve", "tensor_scalar", sM[:, :], sM[:, :], fm["f"][:, t:t + 1], None, ALU.mult)
                kb.op("dve", "scalar_tensor_tensor", sM[:, 0:128], bm[:, i, 128:256], fm["vi"][:, t:t + 1], sM[:, 0:128], ALU.mult, ALU.add)
                kb.op("dve", "scalar_tensor_tensor", sM[:, 128:256], bm[:, i, 128:256], fm["ei"][:, t:t + 1], sM[:, 128:256], ALU.mult, ALU.add)
                kb.op("dve", "scalar_tensor_tensor", junk[:, :], sM[:, 0:128], 1.0, bm[:, i, 0:128], ALU.mult, ALU.mult,
                      accum_out=fm["num"][:, t:t + 1])
                kb.op("dve", "scalar_tensor_tensor", junk[:, :], sM[:, 128:256], 1.0, bm[:, i, 0:128], ALU.mult, ALU.mult,
                      accum_out=fm["den"][:, t:t + 1])
                kb.op("dve", "tensor_scalar", sS[:, :], sS[:, :], fm["dA"][:, t:t + 1], None, ALU.mult)
                kb.op("dve", "scalar_tensor_tensor", sS[:, :], bs_[:, i, 0:128], fm["dtx"][:, t:t + 1], sS[:, :], ALU.mult, ALU.add)
                kb.op("dve", "scalar_tensor_tensor", junk[:, :], sS[:, :], 1.0, bs_[:, i, 128:256], ALU.mult, ALU.mult,
                      accum_out=fm["y"][:, t:t + 1])
        kb.op("dve", "scalar_tensor_tensor", fm["tmp"][:, :], fm["den"][:, :], -1.0, fm["den"][:, :], ALU.mult, ALU.max)
        kb.op("dve", "tensor_scalar", fm["den"][:, :], fm["tmp"][:, :], 1.0, None, ALU.max)
        kb.op("dve", "reciprocal", fm["den"][:, :], fm["den"][:, :])
        kb.op("dve", "tensor_tensor", fm["o1"][:, :], fm["num"][:, :], fm["den"][:, :], ALU.mult)
        kb.dma("sp", outT[0:128, ts], fm["o1"][:, :])
        kb.op("dve", "scalar_tensor_tensor", fm["o2"][:, :], fm["xc"][:, :], cols[:, 19:20], fm["y"][:, :], ALU.mult, ALU.add)
        kb.op("dve", "tensor_tensor", fm["o2"][:, :], fm["o2"][:, :], fm["zs"][:, :], ALU.mult)
        kb.dma("sp", outT[256:384, ts], fm["o2"][:, :])
    return kb.finish()


def lay16(v):
    return np.ascontiguousarray(v.reshape(16, 128).T)

def prep_a_even(d, j, layer, hT):
    w_in = d['w_in_even'][j]; mu = d['rw_mu'][j]
    maps = []
    for c in range(8):
        s = slice(c * 128, (c + 1) * 128)
        cs = lambda base: w_in[:, base + c * 128: base + (c + 1) * 128]
        Wc = np.concatenate([cs(0), cs(1024), cs(4096), cs(4096 + 1024), cs(2048), cs(3072), cs(4096 + 2048),
                             w_in[:, 4096 + 3072:4096 + 3136], w_in[:, 4096 + 3136:4096 + 3200],
                             w_in[:, 4096 + 3200:4096 + 3328], w_in[:, 4096 + 3328:4096 + 3360]], axis=1)
        vecs = np.concatenate([d['hg_lb_table'][:, s].reshape(-1), mu[0:1024][s], mu[1024:2048][s],
                               d['rw_w0'][j][s], d['rw_a0'][j][s], d['rw_k_k'][j][s], d['rw_k_a'][j][s],
                               d['rw_r_k'][j].reshape(-1)[s],
                               np.repeat((np.arange(4) >= 1) & (np.arange(4) <= layer), 128).astype(np.float32)])[None, :]
        cols = np.zeros((128, 8), np.float32)
        cols[:, 0] = d['hg_norm'][j][s]; cols[:, 1] = mu[2048:3072][s]
        cols[:, 2] = mu[3072:3200]; cols[:, 3] = mu[3200:3328]; cols[0:32, 4] = mu[3328:3360]
        cols[:, 5] = d['rw_ln_w'][j][s]; cols[:, 6] = d['rw_ln_b'][j][s]
        lora = np.concatenate([d['rw_w2'][j][:, s], d['rw_a2'][j][:, s]], axis=0)
        maps.append({"hT": hT, "g": lay16(d['norm_mix'][layer]), "Wc": np.ascontiguousarray(Wc),
                     "vecs": np.ascontiguousarray(vecs.astype(np.float32)), "cols": cols,
                     "lora": np.ascontiguousarray(lora), "g2c": np.ascontiguousarray(d['rw_g2'][j][:, s])})
    return maps

def gather_a_even(results):
    mixT = np.zeros((2048, 8192), np.float32)
    for c, r in enumerate(results):
        o = r["outT"]
        mixT[c * 128:(c + 1) * 128] = o[0:128]
        mixT[1024 + c * 128:1024 + (c + 1) * 128] = o[128:256]
    return mixT


def prep_a_odd(d, j, layer, hT):
    w = d['w_in_odd'][j]
    MB = 3080
    cw = d['mb_conv_w'][j]; cbv = d['mb_conv_b'][j]
    maps = []
    for c in range(8):
        hd, half, gi = c // 2, c % 2, c // 4
        rep = lambda col: np.repeat(w[:, col:col + 1], 128, axis=1)
        vcol = 1024 + hd * 256 + half * 128
        ocol = 2056 + hd * 256 + half * 128
        dtrep = np.concatenate([np.repeat(w[:, MB + 2560 + 2 * c:MB + 2560 + 2 * c + 1], 64, axis=1),
                                np.repeat(w[:, MB + 2560 + 2 * c + 1:MB + 2560 + 2 * c + 2], 64, axis=1)], axis=1)
        xch = slice(c * 128, (c + 1) * 128)
        bch = slice(1024 + gi * 128, 1024 + (gi + 1) * 128)
        cch = slice(1280 + gi * 128, 1280 + (gi + 1) * 128)
        Wc = np.concatenate([w[:, hd * 128:(hd + 1) * 128], w[:, 512 + hd * 128:512 + (hd + 1) * 128],
                             w[:, vcol:vcol + 128], rep(2048 + hd), rep(2052 + hd), w[:, ocol:ocol + 128],
                             w[:, MB + c * 128:MB + (c + 1) * 128],
                             w[:, MB + 1024 + xch.start:MB + 1024 + xch.stop],
                             w[:, MB + 1024 + bch.start:MB + 1024 + bch.stop],
                             w[:, MB + 1024 + cch.start:MB + 1024 + cch.stop], dtrep], axis=1)
        cols = np.zeros((128, 24), np.float32)
        cols[:, 0] = d['ml_i_bias'][j][hd]; cols[:, 1] = d['ml_f_bias'][j][hd]
        cols[:, 2:6] = cw[:, xch].T; cols[:, 6] = cbv[xch]
        cols[:, 7:11] = cw[:, bch].T; cols[:, 11] = cbv[bch]
        cols[:, 12:16] = cw[:, cch].T; cols[:, 16] = cbv[cch]
        hl = np.repeat(np.array([2 * c, 2 * c + 1]), 64)
        cols[:, 17] = d['mb_dt_bias'][j][hl]; cols[:, 18] = d['mb_A_log'][j][hl]; cols[:, 19] = d['mb_D'][j][hl]
        maps.append({"hT": hT, "g": lay16(d['norm_mix'][layer]), "Wc": np.ascontiguousarray(Wc), "cols": cols})
    return maps


def gather_a_odd(results):
    mixT = np.zeros((2048, 8192), np.float32)
    gateT = np.zeros((1024, 8192), np.float32)
    for c, r in enumerate(results):
        o = r["outT"]
        mixT[c * 128:(c + 1) * 128] = o[0:128]
        gateT[c * 128:(c + 1) * 128] = o[128:256]
        mixT[1024 + c * 128:1024 + (c + 1) * 128] = o[256:384]
    return mixT, gateT


from concourse.bass_utils import run_bass_kernel_spmd

B_NTILE = 4
B_NCORE = 16 // B_NTILE


def kernel(**inp):
    d = {k: np.asarray(v) for k, v in inp.items()}
    hT = np.ascontiguousarray(d['x'][0].T)
    T = B_NTILE * TT
    for layer in range(4):
        j = layer // 2
        odd = layer % 2 == 1
        if not odd:
            res = run_bass_kernel_spmd(build_a_even(), prep_a_even(d, j, layer, hT), core_ids=list(range(8)))
            mixT = gather_a_even(res.results)
            w_out = d['w_out_even'][j]
        else:
            res = run_bass_kernel_spmd(build_a_odd(), prep_a_odd(d, j, layer, hT), core_ids=list(range(8)))
            mixT, gateT = gather_a_odd(res.results)
            w_out = d['w_out_odd'][j]
        del res
        maps = []
        for c in range(B_NCORE):
            m = {"hT": np.ascontiguousarray(hT[:, c * T:(c + 1) * T]),
                 "mixT": np.ascontiguousarray(mixT[:, c * T:(c + 1) * T]),
                 "w_out": w_out, "g_ffn": lay16(d['norm_ffn'][layer]),
                 "w_up": d['ffn_w_up'][layer], "w_dn": d['ffn_w_down'][layer]}
            if odd:
                m["gateT"] = np.ascontiguousarray(gateT[:, c * T:(c + 1) * T])
                m["gmix"] = lay16(np.concatenate([d['ml_norm'][j], d['mb_norm'][j]]))
            maps.append(m)
        res = run_bass_kernel_spmd(build_b(B_NTILE, odd=odd, final=False), maps, core_ids=list(range(B_NCORE)))
        hT = np.ascontiguousarray(np.concatenate([r["outT"] for r in res.results], axis=1))
        del res
    TF = 2 * TT
    maps = [{"hT": np.ascontiguousarray(hT[:, c * TF:(c + 1) * TF]), "g_fin": lay16(d['norm_final'])} for c in range(8)]
    res = run_bass_kernel_spmd(build_f(2), maps, core_ids=list(range(8)))
    yT = np.concatenate([r["outT"] for r in res.results], axis=1)
    return np.ascontiguousarray(yT.T)[None].astype(np.float32)
```

```python
import numpy as np
import concourse.bass as bass
import concourse.mybir as mybir
from concourse.alu_op_type import AluOpType as ALU

F32 = mybir.dt.float32
BF16 = mybir.dt.bfloat16
AF = mybir.ActivationFunctionType
AX = mybir.AxisListType


class KB:
    def __init__(self):
        self.nc = bass.Bass("TRN2", target_bir_lowering=False)
        nc = self.nc
        self.eng = {"pe": nc.tensor, "dve": nc.vector, "act": nc.scalar, "pool": nc.gpsimd, "sp": nc.sync}
        self.LIMIT = 8000
        self.epoch = {e: 0 for e in self.eng}
        self.sem = {(e, 0): nc.alloc_semaphore(name="s_" + e + "0") for e in self.eng}
        self.cnt = {e: 0 for e in self.eng}
        self.dsem = {}
        self.dcnt = {}
        self.seen = {e: {} for e in self.eng}
        self.recs = {}
        self.n_ps = 0
        self.out_dma = []
        self.scr_rr = {}

    def sb(self, name, shape, dt=F32):
        return self.nc.alloc_sbuf_tensor(name, list(shape), dt)

    def ps(self, name, shape, dt=F32):
        return self.nc.alloc_psum_tensor(name, list(shape), dt)

    def dram(self, name, shape, dt=F32, kind="ExternalInput"):
        return self.nc.dram_tensor(name, list(shape), dt, kind=kind).ap()

    @staticmethod
    def _region(ap):
        a = ap.ap
        pstep, pcount = a[0]
        off = ap.offset
        if ap.space == "DRAM" or str(ap.space) == "DRAM":
            lo = off
            hi = off + sum((c - 1) * abs(s) for s, c in a) + 1
            return (0, 1, lo, hi)
        if pstep == 0:
            p0, f0 = 0, off
            return (0, 128, f0, f0 + sum((c - 1) * abs(s) for s, c in a[1:]) + 1)
        p0 = off // pstep
        f0 = off % pstep
        hi = f0 + sum((c - 1) * abs(s) for s, c in a[1:]) + 1
        return (p0, p0 + pcount, f0, hi)

    @staticmethod
    def _overlap(r1, r2):
        return r1[0] < r2[1] and r2[0] < r1[1] and r1[2] < r2[3] and r2[2] < r1[3]

    @staticmethod
    def _contains(big, small):
        return big[0] <= small[0] and big[1] >= small[1] and big[2] <= small[2] and big[3] >= small[3]

    def _deps(self, e, reads, writes):
        need = {}
        for ap in reads:
            reg = self._region(ap)
            for (r, sk, isw), c in self.recs.get(ap.tensor.name, {}).items():
                if isw and self._overlap(reg, r):
                    if isinstance(sk, tuple) and sk[0] == e and e == "pe":
                        continue
                    need[sk] = max(need.get(sk, 0), c)
        for ap in writes:
            reg = self._region(ap)
            for (r, sk, isw), c in self.recs.get(ap.tensor.name, {}).items():
                if self._overlap(reg, r):
                    if isinstance(sk, tuple) and sk[0] == e and (e == "pe" or not isw):
                        continue
                    need[sk] = max(need.get(sk, 0), c)
        return need

    def _semof(self, sk):
        return self.sem[sk] if sk in self.sem else self.dsem[sk]

    def _emit_waits(self, e, need):
        for sk, c in need.items():
            if self.seen[e].get(sk, 0) < c:
                self.eng[e].wait_ge(self._semof(sk), c)
                self.seen[e][sk] = c

    def _record(self, sk, c, reads, writes):
        for ap in reads:
            d = self.recs.setdefault(ap.tensor.name, {})
            d[(self._region(ap), sk, False)] = c
        for ap in writes:
            d = self.recs.setdefault(ap.tensor.name, {})
            reg = self._region(ap)
            for k in [k for k in d if self._contains(reg, k[0])]:
                del d[k]
            d[(reg, sk, True)] = c

    def op(self, e, fn, *args, reads=None, writes=None, **kw):
        aps_in = []
        out = kw.get("out", None)
        lst = list(args) + [v for k, v in kw.items() if k != "out"]
        if out is None:
            out = args[0]
            lst = list(args[1:]) + list(kw.values())
        for a in lst:
            if isinstance(a, bass.AP):
                aps_in.append(a)
        if reads is None:
            reads = aps_in
        if writes is None:
            writes = [out]
            if isinstance(kw.get("accum_out", None), bass.AP):
                writes.append(kw["accum_out"])
                reads = [a for a in reads if a is not kw["accum_out"]]
        need = self._deps(e, reads, writes)
        self._emit_waits(e, need)
        if self.cnt[e] >= self.LIMIT:
            self.epoch[e] += 1
            self.cnt[e] = 0
            self.sem[(e, self.epoch[e])] = self.nc.alloc_semaphore(name="s_" + e + str(self.epoch[e]))
        ins = getattr(self.eng[e], fn)(*args, **kw)
        self.cnt[e] += 1
        sk = (e, self.epoch[e])
        ins.then_inc(self.sem[sk], 1)
        self._record(sk, self.cnt[e], reads, writes)
        return ins

    def dma(self, q, out, in_, **kw):
        need = self._deps("dma", [in_], [out])
        self._emit_waits(q, need)
        tn = out.tensor.name
        if str(out.space) == "DRAM":
            if tn.startswith("scr"):
                i = self.scr_rr.get(tn, 0)
                self.scr_rr[tn] = i + 1
                name = "d_%s_r%d" % (tn, i % 8)
            else:
                name = "d_" + tn
        else:
            name = "d_%s_%s" % (tn, "_".join(str(v) for v in self._region(out)))
        if name not in self.dsem:
            self.dsem[name] = self.nc.alloc_semaphore(name=name)
            self.dcnt[name] = 0
        ins = self.eng[q].dma_start(out=out, in_=in_, **kw)
        self.dcnt[name] += 16
        ins.then_inc(self.dsem[name], 16)
        self._record(name, self.dcnt[name], [in_], [out])
        if str(out.space) == "DRAM":
            self.out_dma.append((name, self.dcnt[name]))
        return ins

    def finish(self):
        last = {}
        for sk, c in self.out_dma:
            last[sk] = max(last.get(sk, 0), c)
        for sk, c in last.items():
            self.eng["sp"].wait_ge(self.dsem[sk], c)
        return self.nc


EPS = 1e-6
NT = 1024


def rmsnorm_fm(kb, h_sb, g_sb, uT, ones32, sq, rstd, pss, nt=NT):
    nh = nt // 512
    for c in range(16):
        kb.op("act", "activation", sq[:, c % 2, :], h_sb[:, c, :], AF.Square)
        for j in range(nh):
            kb.op("pe", "matmul", pss[j][:, :], ones32[:, :], sq[:, c % 2, j * 512:(j + 1) * 512],
                  start=(c == 0), stop=(c == 15))
    for j in range(nh):
        kb.op("act", "activation", rstd[:, j * 512:(j + 1) * 512], pss[j][:, :], AF.Sqrt,
              bias=kb.eps_ap, scale=1.0 / 2048.0)
    kb.op("dve", "reciprocal", rstd[:, :], rstd[:, :])
    for c in range(16):
        kb.op("dve", "scalar_tensor_tensor", uT[:, c, :], h_sb[:, c, :], g_sb[:, c:c + 1], rstd[:, :],
              ALU.mult, ALU.mult)


def consts(kb):
    kb.eps_t = kb.sb("eps_t", [128, 1])
    kb.op("pool", "memset", kb.eps_t[:, :], EPS)
    kb.eps_ap = kb.eps_t[:, 0:1]


def build_d1(E):
    kb = KB()
    hT = kb.dram("hT", [2048, NT])
    g = kb.dram("g", [128, 16])
    W = kb.dram("W", [2048, E])
    p = kb.dram("p", [NT, E], kind="ExternalOutput")
    consts(kb)
    h_sb = kb.sb("h_sb", [128, 16, NT])
    uT = kb.sb("uT", [128, 16, NT], BF16)
    g_sb = kb.sb("g_sb", [128, 16])
    ones32 = kb.sb("ones32", [128, 128])
    sq = kb.sb("sq", [128, 2, NT])
    rstd = kb.sb("rstd", [128, NT])
    wb = [kb.sb(f"wb{i}", [128, 16, 512], BF16) for i in range(2)]
    ob = [kb.sb(f"ob{i}", [128, 512]) for i in range(4)]
    pss = [kb.ps(f"ps{i}", [128, 512]) for i in range(8)]
    kb.op("pool", "memset", ones32[:, :], 1.0)
    kb.dma("sp", g_sb[:, :], g[:, :])
    hv = hT.rearrange("(c p) t -> p c t", p=128)
    for c in range(0, 16, 4):
        kb.dma("sp", h_sb[:, c:c + 4, :], hv[:, c:c + 4, :])
    rmsnorm_fm(kb, h_sb, g_sb, uT, ones32, sq, rstd, pss[0:2])
    Wv = W.rearrange("(c p) n -> p c n", p=128)
    nb = (E + 511) // 512
    k = 0
    for b in range(nb):
        n0 = b * 512
        n = min(512, E - n0)
        w = wb[b % 2]
        for c0 in range(0, 16, 4):
            kb.dma("pool", w[:, c0:c0 + 4, 0:n], Wv[:, c0:c0 + 4, n0:n0 + n])
        for m in range(NT // 128):
            pt = pss[2 + k % 6]
            o = ob[k % 4]
            for c in range(16):
                kb.op("pe", "matmul", pt[:, 0:n], uT[:, c, m * 128:(m + 1) * 128], w[:, c, 0:n],
                      start=(c == 0), stop=(c == 15))
            if k % 2 == 0:
                kb.op("dve", "tensor_copy", o[:, 0:n], pt[:, 0:n])
            else:
                kb.op("act", "copy", o[:, 0:n], pt[:, 0:n])
            kb.dma("sp", p[m * 128:(m + 1) * 128, n0:n0 + n], o[:, 0:n])
            k += 1
    return kb.finish()


TT = 512


def rmsnorm_fm2(kb, h_sb, g_sb, out, ones32, sq, rstd, ps, nchunk=16, width=2048.0, eps=EPS, gate=None):
    for c in range(nchunk):
        kb.op("act", "activation", sq[:, c % 2, :], h_sb[:, c, :], AF.Square)
        kb.op("pe", "matmul", ps[:, :], ones32[:, :], sq[:, c % 2, :], start=(c == 0), stop=(c == nchunk - 1))
    kb.op("act", "activation", rstd[:, :], ps[:, :], AF.Sqrt, bias=kb.eps_ap if eps == EPS else eps, scale=1.0 / width)
    kb.op("dve", "reciprocal", rstd[:, :], rstd[:, :])
    for c in range(nchunk):
        if gate is None:
            kb.op("dve", "scalar_tensor_tensor", out[:, c, :], h_sb[:, c, :], g_sb[:, c:c + 1], rstd[:, :],
                  ALU.mult, ALU.mult)
        else:
            kb.op("dve", "scalar_tensor_tensor", sq[:, 0, :], h_sb[:, c, :], g_sb[:, c:c + 1], rstd[:, :],
                  ALU.mult, ALU.mult)
            kb.op("pool", "tensor_tensor", out[:, c, :], sq[:, 0, :], gate[:, c, :], ALU.mult)


def build_b(ntile, odd=False, final=False):
    kb = KB()
    T = ntile * TT
    DFF = 5632
    hT = kb.dram("hT", [2048, T])
    mixT = kb.dram("mixT", [2048, T])
    if odd:
        gateT = kb.dram("gateT", [1024, T])
        gmix = kb.dram("gmix", [128, 16])
    w_out = kb.dram("w_out", [2048, 2048])
    g_ffn = kb.dram("g_ffn", [128, 16])
    w_up = kb.dram("w_up", [2048, 2 * DFF])
    w_dn = kb.dram("w_dn", [DFF, 2048])
    if final:
        g_fin = kb.dram("g_fin", [128, 16])
    outT = kb.dram("outT", [2048, T], kind="ExternalOutput")
    consts(kb)
    h_sb = kb.sb("h_sb", [128, 16, TT])
    uT = kb.sb("uT", [128, 16, TT], BF16)
    act = kb.sb("act", [128, 44, TT], BF16)
    g_sb = kb.sb("g_sb", [128, 16])
    gf_sb = kb.sb("gf_sb", [128, 16])
    gm_sb = kb.sb("gm_sb", [128, 16])
    ones32 = kb.sb("ones32", [128, 128])
    sq = kb.sb("sq", [128, 2, TT])
    rstd = kb.sb("rstd", [128, TT])
    sg = [kb.sb(f"sg{i}", [128, TT]) for i in range(2)]
    NWB = 2 if odd else 3
    wbuf = [kb.sb(f"wb{i}", [128, 11264], BF16) for i in range(NWB)]
    if odd:
        mraw = kb.sb("mraw", [128, 4, TT])
        gate_sb = kb.sb("gate_sb", [128, 2, TT])
    pss = [kb.ps(f"ps{i}", [128, 512]) for i in range(8)]
    kb.op("pool", "memset", ones32[:, :], 1.0)
    kb.dma("sp", g_sb[:, :], g_ffn[:, :])
    if final:
        kb.dma("sp", gf_sb[:, :], g_fin[:, :])
    if odd:
        kb.dma("sp", gm_sb[:, :], gmix[:, :])
    hv = hT.rearrange("(c p) t -> p c t", p=128)
    mv = mixT.rearrange("(c p) t -> p c t", p=128)
    ov = outT.rearrange("(c p) t -> p c t", p=128)
    wov = w_out.rearrange("(c p) n -> p c n", p=128)
    wuv = w_up.rearrange("(c p) n -> p c n", p=128)
    wdv = w_dn.rearrange("(f p) n -> p f n", p=128)
    wk = 0
    pk = 0
    for t in range(ntile):
        ts = slice(t * TT, (t + 1) * TT)
        for c in range(0, 16, 8):
            kb.dma("sp", h_sb[:, c:c + 8, :], hv[:, c:c + 8, ts])
        if not odd:
            for c in range(0, 16, 8):
                kb.dma("pool", uT[:, c:c + 8, :], mv[:, c:c + 8, ts])
        else:
            gv = gateT.rearrange("(c p) t -> p c t", p=128)
            for hd in range(4):
                kb.dma("sp", mraw[:, 0:2, :], mv[:, 2 * hd:2 * hd + 2, ts])
                kb.dma("sp", gate_sb[:, :, :], gv[:, 2 * hd:2 * hd + 2, ts])
                rmsnorm_fm2(kb, mraw[:, 0:2, :], gm_sb[:, 2 * hd:2 * hd + 2],
                            uT[:, 2 * hd:2 * hd + 2, :], ones32, sq, rstd, pss[0], nchunk=2, width=256.0,
                            gate=gate_sb[:, 0:2, :])
            for gp in range(2):
                kb.dma("sp", mraw[:, 0:4, :], mv[:, 8 + 4 * gp:12 + 4 * gp, ts])
                rmsnorm_fm2(kb, mraw[:, 0:4, :], gm_sb[:, 8 + 4 * gp:12 + 4 * gp],
                            uT[:, 8 + 4 * gp:12 + 4 * gp, :], ones32, sq, rstd, pss[0], nchunk=4, width=512.0)
        for ob in range(4):
            w = wbuf[wk % NWB]; wk += 1
            w3 = w[:, 0:8192].rearrange("p (c n) -> p c n", c=16)
            for c0 in range(0, 16, 8):
                kb.dma("pool", w3[:, c0:c0 + 8, :], wov[:, c0:c0 + 8, ob * 512:(ob + 1) * 512])
            for j in range(4):
                oc = ob * 4 + j
                pt = pss[1 + pk % 7]; pk += 1
                for c in range(16):
                    kb.op("pe", "matmul", pt[:, :], w3[:, c, j * 128:(j + 1) * 128], uT[:, c, :],
                          start=(c == 0), stop=(c == 15))
                kb.op("dve", "tensor_tensor", h_sb[:, oc, :], h_sb[:, oc, :], pt[:, :], ALU.add)
        rmsnorm_fm2(kb, h_sb, g_sb, uT, ones32, sq, rstd, pss[0])
        for fb in range(11):
            wg = wbuf[wk % NWB]; wk += 1
            wu = wbuf[wk % NWB]; wk += 1
            wg3 = wg[:, 0:8192].rearrange("p (c n) -> p c n", c=16)
            wu3 = wu[:, 0:8192].rearrange("p (c n) -> p c n", c=16)
            for c0 in range(0, 16, 8):
                kb.dma("pool", wg3[:, c0:c0 + 8, :], wuv[:, c0:c0 + 8, fb * 512:(fb + 1) * 512])
            for c0 in range(0, 16, 8):
                kb.dma("pool", wu3[:, c0:c0 + 8, :], wuv[:, c0:c0 + 8, DFF + fb * 512:DFF + (fb + 1) * 512])
            for j in range(4):
                f = fb * 4 + j
                pg = pss[1 + pk % 7]; pk += 1
                pu = pss[1 + pk % 7]; pk += 1
                for c in range(16):
                    kb.op("pe", "matmul", pg[:, :], wg3[:, c, j * 128:(j + 1) * 128], uT[:, c, :],
                          start=(c == 0), stop=(c == 15))
                for c in range(16):
                    kb.op("pe", "matmul", pu[:, :], wu3[:, c, j * 128:(j + 1) * 128], uT[:, c, :],
                          start=(c == 0), stop=(c == 15))
                s = sg[f % 2]
                kb.op("act", "activation", s[:, :], pg[:, :], AF.Silu)
                kb.op("dve", "tensor_tensor", act[:, f, :], s[:, :], pu[:, :], ALU.mult)
        for ob in range(8):
            w = wbuf[wk % NWB]; wk += 1
            w3 = w[:, 0:11264].rearrange("p (f n) -> p f n", f=44)
            for f0 in range(0, 44, 11):
                kb.dma("pool", w3[:, f0:f0 + 11, :], wdv[:, f0:f0 + 11, ob * 256:(ob + 1) * 256])
            for j in range(2):
                oc = ob * 2 + j
                pt = pss[1 + pk % 7]; pk += 1
                for f in range(44):
                    kb.op("pe", "matmul", pt[:, :], w3[:, f, j * 128:(j + 1) * 128], act[:, f, :],
                          start=(f == 0), stop=(f == 43))
                kb.op("dve", "tensor_tensor", h_sb[:, oc, :], h_sb[:, oc, :], pt[:, :], ALU.add)
        if final:
            rmsnorm_fm2(kb, h_sb, gf_sb, h_sb, ones32, sq, rstd, pss[0])
        for c in range(0, 16, 8):
            kb.dma("sp", ov[:, c:c + 8, ts], h_sb[:, c:c + 8, :])
    return kb.finish()


def build_f(ntile):
    kb = KB()
    T = ntile * TT
    hT = kb.dram("hT", [2048, T])
    g_fin = kb.dram("g_fin", [128, 16])
    outT = kb.dram("outT", [2048, T], kind="ExternalOutput")
    consts(kb)
    h_sb = kb.sb("h_sb", [128, 16, TT])
    o_sb = kb.sb("o_sb", [128, 16, TT])
    gf_sb = kb.sb("gf_sb", [128, 16])
    ones32 = kb.sb("ones32", [128, 128])
    sq = kb.sb("sq", [128, 2, TT])
    rstd = kb.sb("rstd", [128, TT])
    ps = kb.ps("ps0", [128, 512])
    kb.op("pool", "memset", ones32[:, :], 1.0)
    kb.dma("sp", gf_sb[:, :], g_fin[:, :])
    hv = hT.rearrange("(c p) t -> p c t", p=128)
    ov = outT.rearrange("(c p) t -> p c t", p=128)
    for t in range(ntile):
        ts = slice(t * TT, (t + 1) * TT)
        for c in range(0, 16, 8):
            kb.dma("sp", h_sb[:, c:c + 8, :], hv[:, c:c + 8, ts])
        rmsnorm_fm2(kb, h_sb, gf_sb, o_sb, ones32, sq, rstd, ps)
        for c in range(0, 16, 8):
            kb.dma("sp", ov[:, c:c + 8, ts], o_sb[:, c:c + 8, :])
    return kb.finish()


import math

TB = 256
SBK = 16
NTOK = 8192


def norm_block(kb, hv, blk, h_blk, g_sb, uT, ones32, sq, rstd, ps, halo):
    ts = slice(blk * TB, (blk + 1) * TB)
    if blk > 0:
        kb.op("pool", "tensor_copy", uT[:, :, 0:halo], uT[:, :, TB:TB + halo])
    for c in range(0, 16, 8):
        kb.dma("sp", h_blk[:, c:c + 8, :], hv[:, c:c + 8, ts])
    for c in range(16):
        kb.op("act", "activation", sq[:, c % 2, :], h_blk[:, c, :], AF.Square)
        kb.op("pe", "matmul", ps[:, 0:TB], ones32[:, :], sq[:, c % 2, :], start=(c == 0), stop=(c == 15))
    kb.op("act", "activation", rstd[:, :], ps[:, 0:TB], AF.Sqrt, bias=kb.eps_ap, scale=1.0 / 2048.0)
    kb.op("dve", "reciprocal", rstd[:, :], rstd[:, :])
    for c in range(16):
        kb.op("pool" if c % 2 else "dve", "scalar_tensor_tensor" if True else "", uT[:, c, halo:halo + TB],
              h_blk[:, c, :], g_sb[:, c:c + 1], rstd[:, :], ALU.mult, ALU.mult) if c % 2 == 0 else \
            kb.op("dve", "scalar_tensor_tensor", uT[:, c, halo:halo + TB], h_blk[:, c, :], g_sb[:, c:c + 1],
                  rstd[:, :], ALU.mult, ALU.mult)


def proj_fm(kb, ps, Wsb, col0, ncol, uT, off, n=TB):
    for c in range(16):
        kb.op("pe", "matmul", ps[0:ncol, 0:n], Wsb[:, c, col0:col0 + ncol], uT[:, c, off:off + n],
              start=(c == 0), stop=(c == 15))


def proj_tm(kb, ps, Wsb, col0, ncol, uT, off):
    for c in range(16):
        kb.op("pe", "matmul", ps[:, 0:ncol], uT[:, c, off:off + 128], Wsb[:, c, col0:col0 + ncol],
              start=(c == 0), stop=(c == 15))


def lerp_fm(kb, out, ps_cur, ps_prev, mu_col, tmp, np_=128, n=TB):
    kb.op("act", "copy", tmp[0:np_, 0:n], ps_cur[0:np_, 0:n])
    kb.op("dve", "tensor_tensor", out[0:np_, 0:n], ps_prev[0:np_, 0:n], tmp[0:np_, 0:n], ALU.subtract)
    kb.op("dve", "scalar_tensor_tensor", out[0:np_, 0:n], out[0:np_, 0:n], mu_col, tmp[0:np_, 0:n], ALU.mult, ALU.add)


def build_a_even():
    kb = KB()
    NC = 1184
    hT = kb.dram("hT", [2048, NTOK])
    g = kb.dram("g", [128, 16])
    Wc = kb.dram("Wc", [2048, NC])
    vecs = kb.dram("vecs", [1, 512 + 256 + 5 * 128 + 512])
    colsd = kb.dram("cols", [128, 8])
    lora = kb.dram("lora", [128, 128])
    g2c = kb.dram("g2c", [160, 128])
    outT = kb.dram("outT", [256, NTOK], kind="ExternalOutput")
    scr_h = kb.dram("scr_h", [NTOK, 384], kind="Internal")
    scr_r = kb.dram("scr_r", [2, NTOK, 320], kind="Internal")
    consts(kb)
    eps2 = kb.sb("eps2", [128, 1]); kb.op("pool", "memset", eps2[:, :], 64e-5)
    halo = 1
    h_blk = kb.sb("h_blk", [128, 16, TB])
    uT = kb.sb("uT", [128, 16, TB + halo], BF16)
    g_sb = kb.sb("g_sb", [128, 16])
    ones32 = kb.sb("ones32", [128, 128])
    blk64 = kb.sb("blk64", [128, 128])
    ident = kb.sb("ident", [128, 128])
    hsel = kb.sb("hsel", [2, 128])
    sq = kb.sb("sq", [128, 2, TB])
    rstd = kb.sb("rstd", [128, TB])
    Wsb = kb.sb("Wsb", [128, 16, NC], BF16)
    vrep = kb.sb("vrep", [128, 512 + 256 + 640 + 512])
    cols = kb.sb("cols_sb", [128, 8])
    lora_sb = kb.sb("lora_sb", [128, 128])
    g2a = kb.sb("g2a", [128, 128]); g2b = kb.sb("g2b", [32, 128])
    lb = kb.sb("lb", [128, 128]); oml = kb.sb("oml", [128, 128]); esum = kb.sb("esum", [128, 128])
    pss = [kb.ps(f"ps{i}", [128, 512]) for i in range(8)]
    kb.op("pool", "memset", ones32[:, :], 1.0)
    kb.op("pool", "memset", blk64[:, :], 0.0)
    kb.op("pool", "memset", blk64[0:64, 0:64], 1.0)
    kb.op("pool", "memset", blk64[64:128, 64:128], 1.0)
    kb.op("pool", "memset", ident[:, :], 1.0)
    kb.op("pool", "affine_select", ident[:, :], ident[:, :], [[-1, 128]], ALU.is_equal, 0.0, base=0, channel_multiplier=1)
    kb.op("pool", "memset", hsel[:, :], 1.0)
    kb.op("pool", "affine_select", hsel[:, :], hsel[:, :], [[1, 128]], ALU.is_ge, 0.0, base=0, channel_multiplier=-64)
    kb.op("pool", "affine_select", hsel[:, :], hsel[:, :], [[-1, 128]], ALU.is_ge, 0.0, base=63, channel_multiplier=64)
    kb.dma("sp", g_sb[:, :], g[:, :])
    kb.dma("sp", cols[:, :], colsd[:, :])
    kb.dma("sp", lora_sb[:, :], lora[:, :])
    kb.dma("sp", g2a[:, :], g2c[0:128, :]); kb.dma("sp", g2b[:, :], g2c[128:160, :])
    kb.dma("sp", vrep[:, :], vecs[0:1, :].partition_broadcast(128))
    Wv = Wc.rearrange("(c p) n -> p c n", p=128)
    for c0 in range(0, 16, 4):
        kb.dma("pool", Wsb[:, c0:c0 + 4, :], Wv[:, c0:c0 + 4, :])
    kb.op("act", "activation", vrep[:, 0:512], vrep[:, 0:512], AF.Exp)
    kb.op("dve", "tensor_tensor", esum[:, :], vrep[:, 0:128], vrep[:, 128:256], ALU.add)
    kb.op("dve", "tensor_tensor", esum[:, :], esum[:, :], vrep[:, 256:384], ALU.add)
    kb.op("dve", "tensor_tensor", esum[:, :], esum[:, :], vrep[:, 384:512], ALU.add)
    kb.op("dve", "reciprocal", esum[:, :], esum[:, :])
    kb.op("dve", "tensor_tensor", vrep[:, 0:512], vrep[:, 0:512], vrep[:, 1408:1920], ALU.mult)
    kb.op("dve", "tensor_tensor", lb[:, :], vrep[:, 0:128], vrep[:, 128:256], ALU.add)
    kb.op("dve", "tensor_tensor", lb[:, :], lb[:, :], vrep[:, 256:384], ALU.add)
    kb.op("dve", "tensor_tensor", lb[:, :], lb[:, :], vrep[:, 384:512], ALU.add)
    kb.op("dve", "tensor_tensor", lb[:, :], lb[:, :], esum[:, :], ALU.mult)
    kb.op("dve", "tensor_scalar", oml[:, :], lb[:, :], -1.0, 1.0, ALU.mult, ALU.add)
    MU = vrep[:, 512:768]; W0 = vrep[:, 768:896]; A0 = vrep[:, 896:1024]; KK = vrep[:, 1024:1152]
    KA = vrep[:, 1152:1280]; RK = vrep[:, 1280:1408]
    stH = [kb.sb(f"stH{i}", [128, 384]) for i in range(2)]
    stR = [kb.sb(f"stR{i}", [128, 2, 320]) for i in range(2)]
    t1 = kb.sb("t1", [128, 256]); t2 = kb.sb("t2", [128, 256]); t3 = kb.sb("t3", [128, 128]); t4 = kb.sb("t4", [128, 128])
    aa = kb.sb("aa", [128, 128]); kkt = kb.sb("kkt", [128, 128]); ss = kb.sb("ss", [128, 2]); junk = kb.sb("junk", [128, 128])
    bs = kb.sb("bs", [128, 2]); bsT = kb.sb("bsT", [2, TB])
    fm = {n: kb.sb("fm_" + n, [128, TB]) for n in ["iT", "sgT", "vT", "wa", "th", "gl0", "gl1", "gate", "bfm", "tmp", "oH", "oR", "cen", "y"]}
    sH = kb.sb("sH", [128, 128]); sR = kb.sb("sR", [128, 64]); sa = kb.sb("sa", [128, 1])
    bH = [kb.sb(f"bH{i}", [128, SBK, 384]) for i in range(2)]
    bR = [kb.sb(f"bR{i}", [128, SBK, 320]) for i in range(2)]
    kb.op("pool", "memset", sH[:, :], 0.0); kb.op("pool", "memset", sR[:, :], 0.0)
    kb.op("pool", "memset", uT[:, :, 0:halo], 0.0)
    hv = hT.rearrange("(c p) t -> p c t", p=128)
    sk = 0
    for blk in range(NTOK // TB):
        T0 = blk * TB
        norm_block(kb, hv, blk, h_blk, g_sb, uT, ones32, sq, rstd, pss[0], halo)
        for st in range(TB // 128):
            o1 = halo + st * 128
            tok0 = T0 + st * 128
            sth = stH[st % 2]; str_ = stR[st % 2]
            pc = pss[1]; pp = pss[2]
            proj_tm(kb, pc, Wsb, 0, 512, uT, o1)
            proj_tm(kb, pp, Wsb, 256, 256, uT, o1 - 1)
            kb.op("act", "activation", sth[:, 256:384], pc[:, 0:128], AF.Silu)
            kb.op("act", "activation", t3[:, :], pc[:, 128:256], AF.Sigmoid)
            kb.op("dve", "tensor_tensor", t3[:, :], t3[:, :], oml[:, :], ALU.mult)
            kb.op("dve", "tensor_tensor", sth[:, 0:128], t3[:, :], lb[:, :], ALU.add)
            kb.op("dve", "tensor_tensor", sth[:, 128:256], oml[:, :], t3[:, :], ALU.subtract)
            kb.dma("sp", scr_h[tok0:tok0 + 128, :], sth[:, :])
            kb.op("act", "copy", t1[:, :], pc[:, 256:512])
            kb.op("dve", "tensor_tensor", t2[:, :], pp[:, 0:256], t1[:, :], ALU.subtract)
            kb.op("dve", "tensor_tensor", t2[:, :], t2[:, :], MU, ALU.mult)
            kb.op("dve", "tensor_tensor", t1[:, :], t1[:, :], t2[:, :], ALU.add)
            R_ = t1[:, 0:128]; K_ = t1[:, 128:256]
            pc2 = pss[3]; pp2 = pss[4]
            proj_fm(kb, pc2, Wsb, 896, 128, uT, o1, 128)
            proj_fm(kb, pp2, Wsb, 896, 128, uT, o1 - 1, 128)
            lerp_fm(kb, fm["wa"], pc2, pp2, cols[:, 2:3], fm["tmp"], 128, 128)
            kb.op("act", "activation", fm["th"][0:64, 0:128], fm["wa"][0:64, 0:128], AF.Tanh)
            pw = pss[5]; pa = pss[6]
            kb.op("pe", "matmul", pw[:, 0:128], fm["th"][0:64, 0:128], lora_sb[0:64, :], start=True, stop=True)
            kb.op("pe", "matmul", pa[:, 0:128], fm["wa"][64:128, 0:128], lora_sb[64:128, :], start=True, stop=True)
            str3 = str_[:, :, :]
            def slot(i):
                return str_[:, :, i * 64:(i + 1) * 64]
            def v3(ap):
                return ap.rearrange("p (h k) -> p h k", h=2)
            kb.op("dve", "tensor_tensor", t3[:, :], pw[:, 0:128], W0, ALU.add)
            kb.op("act", "activation", t3[:, :], t3[:, :], AF.Sigmoid)
            kb.op("act", "activation", slot(3), v3(t3[:, :]), AF.Exp, scale=-math.exp(-0.5))
            kb.op("dve", "tensor_tensor", aa[:, :], pa[:, 0:128], A0, ALU.add)
            kb.op("act", "activation", aa[:, :], aa[:, :], AF.Sigmoid)
            kb.op("dve", "tensor_tensor", kkt[:, :], K_, KK, ALU.mult)
            for h in range(2):
                kb.op("dve", "scalar_tensor_tensor", junk[:, 0:64], kkt[:, h * 64:(h + 1) * 64], 1.0,
                      kkt[:, h * 64:(h + 1) * 64], ALU.mult, ALU.mult, accum_out=ss[:, h:h + 1])
            kb.op("act", "activation", ss[:, :], ss[:, :], AF.Sqrt)
            kb.op("dve", "tensor_scalar", ss[:, :], ss[:, :], 1e-12, None, ALU.max)
            kb.op("dve", "reciprocal", ss[:, :], ss[:, :])
            kb.op("dve", "tensor_scalar", ss[:, :], ss[:, :], -1.0, None, ALU.mult)
            for h in range(2):
                kb.op("dve", "tensor_scalar", str_[:, h, 0:64], kkt[:, h * 64:(h + 1) * 64], ss[:, h:h + 1], None, ALU.mult)
            kb.op("dve", "scalar_tensor_tensor", slot(1), slot(0), -1.0, v3(aa[:, :]), ALU.mult, ALU.mult)
            kb.op("dve", "scalar_tensor_tensor", t4[:, :], aa[:, :], -1.0, KA, ALU.add, ALU.mult)
            kb.op("dve", "scalar_tensor_tensor", t4[:, :], t4[:, :], 1.0, K_, ALU.add, ALU.mult)
            kb.op("act", "copy", slot(2), v3(t4[:, :]))
            kb.op("act", "copy", slot(4), v3(R_))
            for h in range(2):
                kb.dma("sp", scr_r[h, tok0:tok0 + 128, :], str_[:, h, :])
            kb.op("dve", "tensor_tensor", t3[:, :], R_, RK, ALU.mult)
            for h in range(2):
                kb.op("dve", "scalar_tensor_tensor", junk[:, 0:64], t3[:, h * 64:(h + 1) * 64], 1.0,
                      t4[:, h * 64:(h + 1) * 64], ALU.mult, ALU.mult, accum_out=bs[:, h:h + 1])
            pT = pss[7]
            kb.op("pe", "transpose", pT[0:2, 0:128], bs[:, :], ident[:, :])
            kb.op("act", "copy", bsT[:, st * 128:(st + 1) * 128], pT[0:2, 0:128])
        o0 = halo
        p1 = pss[1]; p2 = pss[2]
        proj_fm(kb, p1, Wsb, 512, 128, uT, o0)
        kb.op("act", "copy", fm["iT"][:, :], p1[:, 0:TB])
        proj_fm(kb, p2, Wsb, 640, 128, uT, o0)
        kb.op("act", "activation", fm["sgT"][:, :], p2[:, 0:TB], AF.Sigmoid)
        p3 = pss[3]; p4 = pss[4]
        proj_fm(kb, p3, Wsb, 768, 128, uT, o0); proj_fm(kb, p4, Wsb, 768, 128, uT, o0 - 1)
        lerp_fm(kb, fm["vT"], p3, p4, cols[:, 1:2], fm["tmp"])
        p5 = pss[5]; p6 = pss[6]
        proj_fm(kb, p5, Wsb, 1024, 128, uT, o0); proj_fm(kb, p6, Wsb, 1024, 128, uT, o0 - 1)
        lerp_fm(kb, fm["gl0"], p5, p6, cols[:, 3:4], fm["tmp"])
        kb.op("act", "activation", fm["gl0"][:, :], fm["gl0"][:, :], AF.Sigmoid)
        proj_fm(kb, p3, Wsb, 1152, 32, uT, o0); proj_fm(kb, p4, Wsb, 1152, 32, uT, o0 - 1)
        lerp_fm(kb, fm["gl1"], p3, p4, cols[0:32, 4:5], fm["tmp"], 32)
        kb.op("act", "activation", fm["gl1"][0:32, :], fm["gl1"][0:32, :], AF.Sigmoid)
        kb.op("pe", "matmul", p5[:, 0:TB], g2a[:, :], fm["gl0"][:, :], start=True, stop=False)
        kb.op("pe", "matmul", p5[:, 0:TB], g2b[:, :], fm["gl1"][0:32, :], start=False, stop=True)
        kb.op("act", "copy", fm["gate"][:, :], p5[:, 0:TB])
        kb.op("pe", "matmul", p6[:, 0:TB], hsel[:, :], bsT[:, :], start=True, stop=True)
        kb.op("dve", "tensor_tensor", fm["bfm"][:, :], p6[:, 0:TB], fm["vT"][:, :], ALU.mult)
        for sb in range(TB // SBK):
            tok0 = T0 + sb * SBK
            bh = bH[sk % 2]; br = bR[sk % 2]; sk += 1
            kb.dma("sp", bh[:, :, :], scr_h[tok0:tok0 + SBK, :].partition_broadcast(128))
            for h in range(2):
                kb.dma("sp", br[h * 64:(h + 1) * 64, :, :], scr_r[h, tok0:tok0 + SBK, :].partition_broadcast(64))
            for i in range(SBK):
                t = sb * SBK + i
                kb.op("dve", "tensor_tensor", sH[:, :], sH[:, :], bh[:, i, 0:128], ALU.mult)
                kb.op("dve", "scalar_tensor_tensor", sH[:, :], bh[:, i, 128:256], fm["iT"][:, t:t + 1], sH[:, :], ALU.mult, ALU.add)
                kb.op("dve", "scalar_tensor_tensor", junk[:, :], sH[:, :], 1.0, bh[:, i, 256:384], ALU.mult, ALU.mult,
                      accum_out=fm["oH"][:, t:t + 1])
                kb.op("dve", "scalar_tensor_tensor", junk[:, 0:64], sR[:, :], 1.0, br[:, i, 0:64], ALU.mult, ALU.mult,
                      accum_out=sa[:, 0:1])
                kb.op("dve", "tensor_tensor", sR[:, :], sR[:, :], br[:, i, 192:256], ALU.mult)
                kb.op("dve", "scalar_tensor_tensor", sR[:, :], br[:, i, 64:128], sa[:, 0:1], sR[:, :], ALU.mult, ALU.add)
                kb.op("dve", "scalar_tensor_tensor", sR[:, :], br[:, i, 128:192], fm["vT"][:, t:t + 1], sR[:, :], ALU.mult, ALU.add)
                kb.op("dve", "scalar_tensor_tensor", junk[:, 0:64], sR[:, :], 1.0, br[:, i, 256:320], ALU.mult, ALU.mult,
                      accum_out=fm["oR"][:, t:t + 1])
        ts = slice(T0, T0 + TB)
        pe1 = pss[1]
        kb.op("act", "activation", sq[:, 0, :], fm["oH"][:, :], AF.Square)
        kb.op("pe", "matmul", pe1[:, 0:TB], ones32[:, :], sq[:, 0, :], start=True, stop=True)
        kb.op("act", "activation", rstd[:, :], pe1[:, 0:TB], AF.Sqrt, bias=kb.eps_ap, scale=1.0 / 128.0)
        kb.op("dve", "reciprocal", rstd[:, :], rstd[:, :])
        kb.op("dve", "scalar_tensor_tensor", fm["y"][:, :], fm["oH"][:, :], cols[:, 0:1], rstd[:, :], ALU.mult, ALU.mult)
        kb.op("dve", "tensor_tensor", fm["y"][:, :], fm["y"][:, :], fm["sgT"][:, :], ALU.mult)
        kb.dma("sp", outT[0:128, ts], fm["y"][:, :])
        pe2 = pss[2]; pe3 = pss[3]
        kb.op("pe", "matmul", pe2[:, 0:TB], blk64[:, :], fm["oR"][:, :], start=True, stop=True)
        kb.op("dve", "scalar_tensor_tensor", fm["cen"][:, :], pe2[:, 0:TB], -1.0 / 64.0, fm["oR"][:, :], ALU.mult, ALU.add)
        kb.op("act", "activation", sq[:, 1, :], fm["cen"][:, :], AF.Square)
        kb.op("pe", "matmul", pe3[:, 0:TB], blk64[:, :], sq[:, 1, :], start=True, stop=True)
        kb.op("act", "activation", rstd[:, :], pe3[:, 0:TB], AF.Sqrt, bias=eps2[:, 0:1], scale=1.0 / 64.0)
        kb.op("dve", "reciprocal", rstd[:, :], rstd[:, :])
        kb.op("dve", "scalar_tensor_tensor", fm["cen"][:, :], fm["cen"][:, :], cols[:, 5:6], rstd[:, :], ALU.mult, ALU.mult)
        kb.op("dve", "scalar_tensor_tensor", fm["cen"][:, :], fm["cen"][:, :], cols[:, 6:7], fm["bfm"][:, :], ALU.add, ALU.add)
        kb.op("dve", "tensor_tensor", fm["cen"][:, :], fm["cen"][:, :], fm["gate"][:, :], ALU.mult)
        kb.dma("sp", outT[128:256, ts], fm["cen"][:, :])
    return kb.finish()


DBG = set()


def build_a_odd():
    kb = KB()
    NC = 1408
    halo = 3
    hT = kb.dram("hT", [2048, NTOK])
    g = kb.dram("g", [128, 16])
    Wc = kb.dram("Wc", [2048, NC])
    colsd = kb.dram("cols", [128, 24])
    outT = kb.dram("outT", [384, NTOK], kind="ExternalOutput")
    scr_m = kb.dram("scr_m", [NTOK, 256], kind="Internal")
    scr_s = kb.dram("scr_s", [NTOK, 256], kind="Internal")
    consts(kb)
    one_c = kb.sb("one_c", [128, 1]); kb.op("pool", "memset", one_c[:, :], 1.0)
    h_blk = kb.sb("h_blk", [128, 16, TB])
    uT = kb.sb("uT", [128, 16, TB + halo], BF16)
    g_sb = kb.sb("g_sb", [128, 16])
    ones32 = kb.sb("ones32", [128, 128])
    ident = kb.sb("ident", [128, 128])
    sq = kb.sb("sq", [128, 2, TB])
    rstd = kb.sb("rstd", [128, TB])
    Wsb = kb.sb("Wsb", [128, 16, NC], BF16)
    cols = kb.sb("cols_sb", [128, 24])
    ibs = kb.sb("ibs", [128, 2]); negA = kb.sb("negA", [128, 1])
    pss = [kb.ps(f"ps{i}", [128, 512]) for i in range(8)]
    kb.op("pool", "memset", ones32[:, :], 1.0)
    kb.op("pool", "memset", ident[:, :], 1.0)
    kb.op("pool", "affine_select", ident[:, :], ident[:, :], [[-1, 128]], ALU.is_equal, 0.0, base=0, channel_multiplier=1)
    kb.dma("sp", g_sb[:, :], g[:, :])
    kb.dma("sp", cols[:, :], colsd[:, :])
    Wv = Wc.rearrange("(c p) n -> p c n", p=128)
    for c0 in range(0, 16, 4):
        kb.dma("pool", Wsb[:, c0:c0 + 4, :], Wv[:, c0:c0 + 4, :])
    kb.op("dve", "tensor_scalar", ibs[:, :], cols[:, 0:2], 1.0 / 15.0, None, ALU.mult)
    kb.op("act", "activation", negA[:, :], cols[:, 18:19], AF.Exp)
    kb.op("dve", "tensor_scalar", negA[:, :], negA[:, :], -1.0, None, ALU.mult)
    stM = [kb.sb(f"stM{i}", [128, 256]) for i in range(2)]
    stS = [kb.sb(f"stS{i}", [128, 256]) for i in range(2)]
    fm = {n: kb.sb("fm_" + n, [128, TB]) for n in ["vT", "ei", "f", "vi", "sgo", "zs", "xc", "Bc", "Cc", "dt", "dA", "dtx",
                                                    "acc", "num", "den", "y", "tmp", "o1", "o2"]}
    junk = kb.sb("junk", [128, 128])
    sM = kb.sb("sM", [128, 256]); sS = kb.sb("sS", [128, 128])
    bM = [kb.sb(f"bM{i}", [128, SBK, 256]) for i in range(2)]
    bS = [kb.sb(f"bS{i}", [128, SBK, 256]) for i in range(2)]
    kb.op("pool", "memset", sM[:, :], 0.0); kb.op("pool", "memset", sS[:, :], 0.0)
    kb.op("pool", "memset", uT[:, :, 0:halo], 0.0)
    hv = hT.rearrange("(c p) t -> p c t", p=128)
    sk = 0

    def conv(dst, col0, cw0, cb):
        ps4 = [pss[1], pss[2], pss[3], pss[4]]
        for j in range(4):
            proj_fm(kb, ps4[j], Wsb, col0, 128, uT, halo - 3 + j)
        kb.op("dve", "tensor_scalar", fm["acc"][:, :], ps4[0][:, 0:TB], cols[:, cw0:cw0 + 1], None, ALU.mult)
        for j in range(1, 4):
            kb.op("dve", "scalar_tensor_tensor", fm["acc"][:, :], ps4[j][:, 0:TB], cols[:, cw0 + j:cw0 + j + 1],
                  fm["acc"][:, :], ALU.mult, ALU.add)
        kb.op("act", "activation", dst[:, :], fm["acc"][:, :], AF.Silu, bias=cols[:, cb:cb + 1])

    for blk in range(NTOK // TB):
        T0 = blk * TB
        ts = slice(T0, T0 + TB)
        norm_block(kb, hv, blk, h_blk, g_sb, uT, ones32, sq, rstd, pss[0], halo)
        for st in (range(TB // 128) if "tm" not in DBG else []):
            o1 = halo + st * 128
            tok0 = T0 + st * 128
            stm = stM[st % 2]
            pc = pss[5]
            proj_tm(kb, pc, Wsb, 0, 256, uT, o1)
            kb.op("dve", "tensor_scalar", stm[:, 0:128], pc[:, 0:128], 128.0 ** -0.5, None, ALU.mult)
            kb.op("dve", "tensor_copy", stm[:, 128:256], pc[:, 128:256])
            kb.dma("sp", scr_m[tok0:tok0 + 128, :], stm[:, :])
        o0 = halo
        p = pss[6]
        proj_fm(kb, p, Wsb, 256, 128, uT, o0)
        kb.op("act", "copy", fm["vT"][:, :], p[:, 0:TB])
        p = pss[7]
        proj_fm(kb, p, Wsb, 384, 128, uT, o0)
        kb.op("act", "activation", fm["ei"][:, :], p[:, 0:TB], AF.Tanh, bias=ibs[:, 0:1], scale=1.0 / 15.0)
        kb.op("act", "activation", fm["ei"][:, :], fm["ei"][:, :], AF.Exp, scale=15.0)
        p = pss[6]
        proj_fm(kb, p, Wsb, 512, 128, uT, o0)
        kb.op("act", "activation", fm["f"][:, :], p[:, 0:TB], AF.Tanh, bias=ibs[:, 1:2], scale=1.0 / 15.0)
        kb.op("act", "activation", fm["f"][:, :], fm["f"][:, :], AF.Sigmoid, scale=15.0)
        p = pss[7]
        proj_fm(kb, p, Wsb, 640, 128, uT, o0)
        kb.op("act", "activation", fm["sgo"][:, :], p[:, 0:TB], AF.Sigmoid)
        kb.dma("sp", outT[128:256, ts], fm["sgo"][:, :])
        kb.op("dve", "tensor_tensor", fm["vi"][:, :], fm["vT"][:, :], fm["ei"][:, :], ALU.mult)
        p = pss[6]
        proj_fm(kb, p, Wsb, 768, 128, uT, o0)
        kb.op("act", "activation", fm["zs"][:, :], p[:, 0:TB], AF.Silu)
        conv(fm["xc"], 896, 2, 6)
        conv(fm["Bc"], 1024, 7, 11)
        conv(fm["Cc"], 1152, 12, 16)
        p = pss[7]
        proj_fm(kb, p, Wsb, 1280, 128, uT, o0)
        kb.op("act", "activation", fm["dt"][:, :], p[:, 0:TB], AF.Exp, bias=cols[:, 17:18])
        kb.op("act", "activation", fm["dt"][:, :], fm["dt"][:, :], AF.Ln, bias=one_c[:, 0:1])
        kb.op("act", "activation", fm["dA"][:, :], fm["dt"][:, :], AF.Exp, scale=negA[:, 0:1])
        kb.op("dve", "tensor_tensor", fm["dtx"][:, :], fm["dt"][:, :], fm["xc"][:, :], ALU.mult)
        for st in (range(TB // 128) if "tr" not in DBG else []):
            tok0 = T0 + st * 128
            sts = stS[st % 2]
            pT = pss[5]
            kb.op("pe", "transpose", pT[:, 0:128], fm["Bc"][:, st * 128:(st + 1) * 128], ident[:, :])
            kb.op("pe", "transpose", pT[:, 128:256], fm["Cc"][:, st * 128:(st + 1) * 128], ident[:, :])
            kb.op("act", "copy", sts[:, :], pT[:, 0:256])
            kb.dma("sp", scr_s[tok0:tok0 + 128, :], sts[:, :])
        for sb in (range(TB // SBK) if "rec" not in DBG else []):
            tok0 = T0 + sb * SBK
            bm = bM[sk % 2]; bs_ = bS[sk % 2]; sk += 1
            kb.dma("sp", bm[:, :, :], scr_m[tok0:tok0 + SBK, :].partition_broadcast(128))
            kb.dma("sp", bs_[:, :, :], scr_s[tok0:tok0 + SBK, :].partition_broadcast(128))
            for i in range(SBK):
                t = sb * SBK + i
                kb.op("dve", "tensor_scalar", sM[:, :], sM[:, :], fm["f"][:, t:t + 1], None, ALU.mult)
                kb.op("dve", "scalar_tensor_tensor", sM[:, 0:128], bm[:, i, 128:256], fm["vi"][:, t:t + 1], sM[:, 0:128], ALU.mult, ALU.add)
                kb.op("dve", "scalar_tensor_tensor", sM[:, 128:256], bm[:, i, 128:256], fm["ei"][:, t:t + 1], sM[:, 128:256], ALU.mult, ALU.add)
                kb.op("dve", "scalar_tensor_tensor", junk[:, :], sM[:, 0:128], 1.0, bm[:, i, 0:128], ALU.mult, ALU.mult,
                      accum_out=fm["num"][:, t:t + 1])
                kb.op("dve", "scalar_tensor_tensor", junk[:, :], sM[:, 128:256], 1.0, bm[:, i, 0:128], ALU.mult, ALU.mult,
                      accum_out=fm["den"][:, t:t + 1])
                kb.op("dve", "tensor_scalar", sS[:, :], sS[:, :], fm["dA"][:, t:t + 1], None, ALU.mult)
                kb.op("dve", "scalar_tensor_tensor", sS[:, :], bs_[:, i, 0:128], fm["dtx"][:, t:t + 1], sS[:, :], ALU.mult, ALU.add)
                kb.op("dve", "scalar_tensor_tensor", junk[:, :], sS[:, :], 1.0, bs_[:, i, 128:256], ALU.mult, ALU.mult,
                      accum_out=fm["y"][:, t:t + 1])
        kb.op("dve", "scalar_tensor_tensor", fm["tmp"][:, :], fm["den"][:, :], -1.0, fm["den"][:, :], ALU.mult, ALU.max)
        kb.op("dve", "tensor_scalar", fm["den"][:, :], fm["tmp"][:, :], 1.0, None, ALU.max)
        kb.op("dve", "reciprocal", fm["den"][:, :], fm["den"][:, :])
        kb.op("dve", "tensor_tensor", fm["o1"][:, :], fm["num"][:, :], fm["den"][:, :], ALU.mult)
        kb.dma("sp", outT[0:128, ts], fm["o1"][:, :])
        kb.op("dve", "scalar_tensor_tensor", fm["o2"][:, :], fm["xc"][:, :], cols[:, 19:20], fm["y"][:, :], ALU.mult, ALU.add)
        kb.op("dve", "tensor_tensor", fm["o2"][:, :], fm["o2"][:, :], fm["zs"][:, :], ALU.mult)
        kb.dma("sp", outT[256:384, ts], fm["o2"][:, :])
    return kb.finish()


def lay16(v):
    return np.ascontiguousarray(v.reshape(16, 128).T)

def prep_a_even(d, j, layer, hT):
    w_in = d['w_in_even'][j]; mu = d['rw_mu'][j]
    maps = []
    for c in range(8):
        s = slice(c * 128, (c + 1) * 128)
        cs = lambda base: w_in[:, base + c * 128: base + (c + 1) * 128]
        Wc = np.concatenate([cs(0), cs(1024), cs(4096), cs(4096 + 1024), cs(2048), cs(3072), cs(4096 + 2048),
                             w_in[:, 4096 + 3072:4096 + 3136], w_in[:, 4096 + 3136:4096 + 3200],
                             w_in[:, 4096 + 3200:4096 + 3328], w_in[:, 4096 + 3328:4096 + 3360]], axis=1)
        vecs = np.concatenate([d['hg_lb_table'][:, s].reshape(-1), mu[0:1024][s], mu[1024:2048][s],
                               d['rw_w0'][j][s], d['rw_a0'][j][s], d['rw_k_k'][j][s], d['rw_k_a'][j][s],
                               d['rw_r_k'][j].reshape(-1)[s],
                               np.repeat((np.arange(4) >= 1) & (np.arange(4) <= layer), 128).astype(np.float32)])[None, :]
        cols = np.zeros((128, 8), np.float32)
        cols[:, 0] = d['hg_norm'][j][s]; cols[:, 1] = mu[2048:3072][s]
        cols[:, 2] = mu[3072:3200]; cols[:, 3] = mu[3200:3328]; cols[0:32, 4] = mu[3328:3360]
        cols[:, 5] = d['rw_ln_w'][j][s]; cols[:, 6] = d['rw_ln_b'][j][s]
        lora = np.concatenate([d['rw_w2'][j][:, s], d['rw_a2'][j][:, s]], axis=0)
        maps.append({"hT": hT, "g": lay16(d['norm_mix'][layer]), "Wc": np.ascontiguousarray(Wc),
                     "vecs": np.ascontiguousarray(vecs.astype(np.float32)), "cols": cols,
                     "lora": np.ascontiguousarray(lora), "g2c": np.ascontiguousarray(d['rw_g2'][j][:, s])})
    return maps

def gather_a_even(results):
    mixT = np.zeros((2048, 8192), np.float32)
    for c, r in enumerate(results):
        o = r["outT"]
        mixT[c * 128:(c + 1) * 128] = o[0:128]
        mixT[1024 + c * 128:1024 + (c + 1) * 128] = o[128:256]
    return mixT


def prep_a_odd(d, j, layer, hT):
    w = d['w_in_odd'][j]
    MB = 3080
    cw = d['mb_conv_w'][j]; cbv = d['mb_conv_b'][j]
    maps = []
    for c in range(8):
        hd, half, gi = c // 2, c % 2, c // 4
        rep = lambda col: np.repeat(w[:, col:col + 1], 128, axis=1)
        vcol = 1024 + hd * 256 + half * 128
        ocol = 2056 + hd * 256 + half * 128
        dtrep = np.concatenate([np.repeat(w[:, MB + 2560 + 2 * c:MB + 2560 + 2 * c + 1], 64, axis=1),
                                np.repeat(w[:, MB + 2560 + 2 * c + 1:MB + 2560 + 2 * c + 2], 64, axis=1)], axis=1)
        xch = slice(c * 128, (c + 1) * 128)
        bch = slice(1024 + gi * 128, 1024 + (gi + 1) * 128)
        cch = slice(1280 + gi * 128, 1280 + (gi + 1) * 128)
        Wc = np.concatenate([w[:, hd * 128:(hd + 1) * 128], w[:, 512 + hd * 128:512 + (hd + 1) * 128],
                             w[:, vcol:vcol + 128], rep(2048 + hd), rep(2052 + hd), w[:, ocol:ocol + 128],
                             w[:, MB + c * 128:MB + (c + 1) * 128],
                             w[:, MB + 1024 + xch.start:MB + 1024 + xch.stop],
                             w[:, MB + 1024 + bch.start:MB + 1024 + bch.stop],
                             w[:, MB + 1024 + cch.start:MB + 1024 + cch.stop], dtrep], axis=1)
        cols = np.zeros((128, 24), np.float32)
        cols[:, 0] = d['ml_i_bias'][j][hd]; cols[:, 1] = d['ml_f_bias'][j][hd]
        cols[:, 2:6] = cw[:, xch].T; cols[:, 6] = cbv[xch]
        cols[:, 7:11] = cw[:, bch].T; cols[:, 11] = cbv[bch]
        cols[:, 12:16] = cw[:, cch].T; cols[:, 16] = cbv[cch]
        hl = np.repeat(np.array([2 * c, 2 * c + 1]), 64)
        cols[:, 17] = d['mb_dt_bias'][j][hl]; cols[:, 18] = d['mb_A_log'][j][hl]; cols[:, 19] = d['mb_D'][j][hl]
        maps.append({"hT": hT, "g": lay16(d['norm_mix'][layer]), "Wc": np.ascontiguousarray(Wc), "cols": cols})
    return maps


def gather_a_odd(results):
    mixT = np.zeros((2048, 8192), np.float32)
    gateT = np.zeros((1024, 8192), np.float32)
    for c, r in enumerate(results):
        o = r["outT"]
        mixT[c * 128:(c + 1) * 128] = o[0:128]
        gateT[c * 128:(c + 1) * 128] = o[128:256]
        mixT[1024 + c * 128:1024 + (c + 1) * 128] = o[256:384]
    return mixT, gateT


from concourse.bass_utils import run_bass_kernel_spmd

B_NTILE = 2
B_NCORE = 16 // B_NTILE


def kernel(**inp):
    d = {k: np.asarray(v) for k, v in inp.items()}
    hT = np.ascontiguousarray(d['x'][0].T)
    T = B_NTILE * TT
    for layer in range(4):
        j = layer // 2
        odd = layer % 2 == 1
        if not odd:
            res = run_bass_kernel_spmd(build_a_even(), prep_a_even(d, j, layer, hT), core_ids=list(range(8)))
            mixT = gather_a_even(res.results)
            w_out = d['w_out_even'][j]
        else:
            res = run_bass_kernel_spmd(build_a_odd(), prep_a_odd(d, j, layer, hT), core_ids=list(range(8)))
            mixT, gateT = gather_a_odd(res.results)
            w_out = d['w_out_odd'][j]
        del res
        maps = []
        for c in range(B_NCORE):
            m = {"hT": np.ascontiguousarray(hT[:, c * T:(c + 1) * T]),
                 "mixT": np.ascontiguousarray(mixT[:, c * T:(c + 1) * T]),
                 "w_out": w_out, "g_ffn": lay16(d['norm_ffn'][layer]),
                 "w_up": d['ffn_w_up'][layer], "w_dn": d['ffn_w_down'][layer]}
            if odd:
                m["gateT"] = np.ascontiguousarray(gateT[:, c * T:(c + 1) * T])
                m["gmix"] = lay16(np.concatenate([d['ml_norm'][j], d['mb_norm'][j]]))
            maps.append(m)
        res = run_bass_kernel_spmd(build_b(B_NTILE, odd=odd, final=False), maps, core_ids=list(range(B_NCORE)))
        hT = np.ascontiguousarray(np.concatenate([r["outT"] for r in res.results], axis=1))
        del res
    TF = 2 * TT
    maps = [{"hT": np.ascontiguousarray(hT[:, c * TF:(c + 1) * TF]), "g_fin": lay16(d['norm_final'])} for c in range(8)]
    res = run_bass_kernel_spmd(build_f(2), maps, core_ids=list(range(8)))
    yT = np.concatenate([r["outT"] for r in res.results], axis=1)
    return np.ascontiguousarray(yT.T)[None].astype(np.float32)
```
